# Optimizing a Trainium2 kernel written in Bass

```python
import math
import jax, jax.numpy as jnp
from jax import lax
import numpy as np

D_MODEL = 4096
BATCH = 1
SEQ = 8192
DEPTH = 2

N_META = 16
CHUNK = 128
NORM_EPS = 1e-6
NEG = -1e30

SSD_HEADS = 32
SSD_HEAD_DIM = 64
SSD_INNER = SSD_HEADS * SSD_HEAD_DIM
SSD_GROUPS = 4
SSD_HPG = SSD_HEADS // SSD_GROUPS
SSD_STATE = 128
SSD_CONV = 5
SSD_CONV_CH = SSD_INNER + 2 * SSD_GROUPS * SSD_STATE

ATT_Q_HEADS = 16
ATT_KV_HEADS = 4
ATT_HEAD_DIM = 128
ATT_WINDOW = 128
ATT_BLOCK = 128

ML_HEADS = 6
ML_QK_DIM = 256
ML_V_DIM = 512
ML_WIDTH = ML_HEADS * ML_V_DIM

FN_GROUPS = 4
FN_GROUP_DIM = 256
FN_WIDTH = FN_GROUPS * FN_GROUP_DIM

N_EXPERTS = 16
EC_FACTOR = 2
EXPERT_FF = 1536

AB_IN_SIZES = (SSD_INNER, SSD_CONV_CH, 2 * SSD_HEADS, ATT_Q_HEADS * ATT_HEAD_DIM, ATT_KV_HEADS * ATT_HEAD_DIM, ATT_KV_HEADS * ATT_HEAD_DIM)
AB_IN_WIDTH = sum(AB_IN_SIZES)
AB_MIX_WIDTH = SSD_INNER + ATT_Q_HEADS * ATT_HEAD_DIM
CD_IN_SIZES = (ML_HEADS * ML_QK_DIM, ML_HEADS * ML_QK_DIM, ML_WIDTH, ML_WIDTH, 2 * ML_HEADS, 2 * ML_HEADS, FN_WIDTH)
CD_IN_WIDTH = sum(CD_IN_SIZES)
CD_MIX_WIDTH = ML_WIDTH + FN_WIDTH

kernel_name = 'hybrid_ssd_swa_mlstm_fnet_ecmoe_encoder'


def split_cols(t, sizes):
    return jnp.split(t, [int(c) for c in np.cumsum(sizes)[:-1]], axis=-1)


def rms_norm(x, w):
    xf = x.astype(jnp.float32)
    y = xf * lax.rsqrt(jnp.mean(xf * xf, axis=-1, keepdims=True) + NORM_EPS)
    return (y * w.astype(jnp.float32)).astype(x.dtype)


def pad_front(t):
    return jnp.pad(t, [(0, 0), (CHUNK - N_META, 0)] + [(0, 0)] * (t.ndim - 2))


def flip_seq(t):
    return jnp.flip(t, axis=1)


def alibi_slopes(n):
    return 2.0 ** (-8.0 * jnp.arange(1, n + 1, dtype=jnp.float32) / n)


def centred_dwconv(u, w, b):
    pad = (SSD_CONV - 1) // 2
    out = lax.conv_general_dilated(u, w[:, None, :].astype(u.dtype), window_strides=(1,), padding=[(pad, pad)],
                                   dimension_numbers=('NWC', 'WIO', 'NWC'), feature_group_count=u.shape[-1])
    return out + b.astype(u.dtype)


def ssd_scan(x, dt, a_coef, bmat, cmat):
    bsz, lp, ng, nr, hp = x.shape
    nc = lp // CHUNK
    a = (dt * a_coef).reshape(bsz, nc, CHUNK, ng, nr).transpose(0, 3, 4, 1, 2)
    a_cum = jnp.cumsum(a, axis=-1)
    xdt = (x * dt[..., None]).reshape(bsz, nc, CHUNK, ng, nr, hp)
    bc = bmat.reshape(bsz, nc, CHUNK, ng, -1)
    cc = cmat.reshape(bsz, nc, CHUNK, ng, -1)
    tri = jnp.tril(jnp.ones((CHUNK, CHUNK), dtype=bool))
    decay_in = jnp.exp(jnp.where(tri, a_cum[..., :, None] - a_cum[..., None, :], NEG))
    cb = jnp.einsum('bclgn,bcsgn->bgcls', cc, bc)
    y_diag = jnp.einsum('bgcls,bgrcls,bcsgrp->bclgrp', cb, decay_in, xdt)
    decay_out = jnp.exp(a_cum[..., -1:] - a_cum)
    states = jnp.einsum('bcsgn,bgrcs,bcsgrp->cbgrpn', bc, decay_out, xdt)
    chunk_decay = jnp.exp(a_cum[..., -1]).transpose(3, 0, 1, 2)

    def step(h, inp):
        dec, st = inp
        return dec[..., None, None] * h + st, h

    _, prev = lax.scan(step, jnp.zeros_like(states[0]), (chunk_decay, states))
    y_off = jnp.einsum('bclgn,cbgrpn,bgrcl->bclgrp', cc, prev, jnp.exp(a_cum))
    return (y_diag + y_off).reshape(bsz, lp, ng, nr, hp)


def ssd_mixer(z, xbc, dt_raw, conv_w, conv_b, a_log, dt_bias, d_skip, norm_w):
    bsz, n_tok, _ = z.shape
    xbc = jax.nn.silu(centred_dwconv(xbc, conv_w, conv_b).astype(jnp.float32))
    xs, bmat, cmat = split_cols(xbc, (SSD_INNER, SSD_GROUPS * SSD_STATE, SSD_GROUPS * SSD_STATE))
    xs = xs.reshape(bsz, n_tok, SSD_GROUPS, SSD_HPG, SSD_HEAD_DIM)
    bmat = bmat.reshape(bsz, n_tok, SSD_GROUPS, SSD_STATE)
    cmat = cmat.reshape(bsz, n_tok, SSD_GROUPS, SSD_STATE)
    dt = jax.nn.softplus(dt_raw.astype(jnp.float32) + dt_bias.astype(jnp.float32))
    dt = dt.reshape(bsz, n_tok, 2, SSD_GROUPS, SSD_HPG)
    a_coef = (-jnp.exp(a_log.astype(jnp.float32))).reshape(2, SSD_GROUPS, SSD_HPG)
    xs_p, b_p, c_p, dt_p = pad_front(xs), pad_front(bmat), pad_front(cmat), pad_front(dt)
    y_f = ssd_scan(xs_p, dt_p[:, :, 0], a_coef[0], b_p, c_p)
    y_b = flip_seq(ssd_scan(flip_seq(xs_p), flip_seq(dt_p[:, :, 1]), a_coef[1], flip_seq(b_p), flip_seq(c_p)))
    y = (y_f + y_b)[:, CHUNK - N_META:] + d_skip.astype(jnp.float32).reshape(SSD_GROUPS, SSD_HPG, 1) * xs
    gate = jax.nn.silu(z.astype(jnp.float32)).reshape(bsz, n_tok, SSD_GROUPS, -1)
    y = y.reshape(bsz, n_tok, SSD_GROUPS, -1) * gate
    y = y * lax.rsqrt(jnp.mean(y * y, axis=-1, keepdims=True) + NORM_EPS)
    y = y.reshape(bsz, n_tok, SSD_INNER) * norm_w.astype(jnp.float32)
    return y.astype(z.dtype)


def window_attention(q, k, v, sink):
    bsz, n_tok, _, hd = q.shape
    grp = ATT_Q_HEADS // ATT_KV_HEADS
    n = n_tok - N_META
    nb = n // ATT_BLOCK
    scale = hd ** -0.5
    q = q.reshape(bsz, n_tok, ATT_KV_HEADS, grp, hd)
    slopes = alibi_slopes(ATT_Q_HEADS).reshape(ATT_KV_HEADS, grp)
    sink_h = sink.astype(jnp.float32).reshape(ATT_KV_HEADS, grp)
    km, vm = k[:, :N_META], v[:, :N_META]
    qb = q[:, N_META:].reshape(bsz, nb, ATT_BLOCK, ATT_KV_HEADS, grp, hd)

    def band(t):
        tp = jnp.pad(t[:, N_META:], ((0, 0), (ATT_BLOCK, ATT_BLOCK), (0, 0), (0, 0)))
        tp = tp.reshape(bsz, nb + 2, ATT_BLOCK, ATT_KV_HEADS, hd)
        return jnp.concatenate([tp[:, :-2], tp[:, 1:-1], tp[:, 2:]], axis=2)

    kb, vb = band(k), band(v)
    qi = jnp.arange(ATT_BLOCK)[:, None]
    kj = jnp.arange(3 * ATT_BLOCK)[None, :]
    dist = jnp.abs(kj - ATT_BLOCK - qi)
    key_pos = (jnp.arange(nb) * ATT_BLOCK - ATT_BLOCK)[:, None, None] + kj[None]
    valid = (dist <= ATT_WINDOW)[None] & (key_pos >= 0) & (key_pos < n)
    s_band = jnp.einsum('bnqhgd,bnkhd->bhgnqk', qb, kb).astype(jnp.float32) * scale
    s_band = jnp.where(valid, s_band - slopes[:, :, None, None, None] * dist, NEG)
    s_meta = jnp.einsum('bnqhgd,bmhd->bhgnqm', qb, km).astype(jnp.float32) * scale
    sink_col = jnp.broadcast_to(sink_h.reshape(1, ATT_KV_HEADS, grp, 1, 1, 1), s_meta.shape[:-1] + (1,))
    p = jax.nn.softmax(jnp.concatenate([s_band, s_meta, sink_col], axis=-1), axis=-1).astype(v.dtype)
    o = (jnp.einsum('bhgnqk,bnkhd->bnqhgd', p[..., :3 * ATT_BLOCK], vb)
         + jnp.einsum('bhgnqm,bmhd->bnqhgd', p[..., 3 * ATT_BLOCK:-1], vm))
    o_real = o.reshape(bsz, n, ATT_Q_HEADS * hd)
    span = N_META + ATT_WINDOW
    dist_m = jnp.abs(jnp.arange(span)[None, :] - jnp.arange(N_META)[:, None])
    s_m = jnp.einsum('bqhgd,bkhd->bhgqk', q[:, :N_META], k[:, :span]).astype(jnp.float32) * scale
    s_m = jnp.where(dist_m <= ATT_WINDOW, s_m, NEG)
    sink_m = jnp.broadcast_to(sink_h.reshape(1, ATT_KV_HEADS, grp, 1, 1), s_m.shape[:-1] + (1,))
    p_m = jax.nn.softmax(jnp.concatenate([s_m, sink_m], axis=-1), axis=-1).astype(v.dtype)
    o_m = jnp.einsum('bhgqk,bkhd->bqhgd', p_m[..., :-1], v[:, :span]).reshape(bsz, N_META, ATT_Q_HEADS * hd)
    return jnp.concatenate([o_m, o_real], axis=1)


def mlstm_scan(q, k, v, log_i, log_f):
    bsz, lp, nh, _ = q.shape
    dv = v.shape[-1]
    nc = lp // CHUNK

    def chunked(t):
        return t.reshape(bsz, nc, CHUNK, nh, -1).transpose(0, 3, 1, 2, 4)

    qc, kc, vc = chunked(q), chunked(k), chunked(v)
    li = log_i.reshape(bsz, nc, CHUNK, nh).transpose(0, 3, 1, 2)
    lf = log_f.reshape(bsz, nc, CHUNK, nh).transpose(0, 3, 1, 2)
    bl = jnp.cumsum(lf, axis=-1)
    g = bl[..., -1]
    tri = jnp.tril(jnp.ones((CHUNK, CHUNK), dtype=bool))
    dmat = jnp.where(tri, bl[..., :, None] - bl[..., None, :] + li[..., None, :], NEG)
    a = g[..., None] - bl + li
    m_loc = jnp.max(a, axis=-1)
    wa = jnp.exp(a - m_loc[..., None])
    c_loc = jnp.einsum('bhcs,bhcsk,bhcsv->cbhkv', wa, kc, vc)
    n_loc = jnp.einsum('bhcs,bhcsk->cbhk', wa, kc)

    def step(carry, inp):
        c_st, n_st, m_st = carry
        g_c, ml_c, cl_c, nl_c = inp
        m_new = jnp.maximum(g_c + m_st, ml_c)
        s_old = jnp.exp(g_c + m_st - m_new)
        s_new = jnp.exp(ml_c - m_new)
        new = (s_old[..., None, None] * c_st + s_new[..., None, None] * cl_c,
               s_old[..., None] * n_st + s_new[..., None] * nl_c, m_new)
        return new, (c_st, n_st, m_st)

    init = (jnp.zeros_like(c_loc[0]), jnp.zeros_like(n_loc[0]), jnp.zeros_like(m_loc[:, :, 0]))
    xs = (g.transpose(2, 0, 1), m_loc.transpose(2, 0, 1), c_loc, n_loc)
    _, (c_prev, n_prev, m_prev) = lax.scan(step, init, xs)
    m_inter = bl + m_prev.transpose(1, 2, 0)[..., None]
    m_t = jnp.maximum(jnp.max(dmat, axis=-1), m_inter)
    p = jnp.exp(dmat - m_t[..., None]) * jnp.einsum('bhcqk,bhcsk->bhcqs', qc, kc)
    w_inter = jnp.exp(m_inter - m_t)
    num = jnp.einsum('bhcqs,bhcsv->bhcqv', p, vc) + w_inter[..., None] * jnp.einsum('bhcqk,cbhkv->bhcqv', qc, c_prev)
    den = jnp.sum(p, axis=-1) + w_inter * jnp.einsum('bhcqk,cbhk->bhcq', qc, n_prev)
    h = num / jnp.maximum(jnp.abs(den), jnp.exp(-m_t))[..., None]
    return h.transpose(0, 2, 3, 1, 4).reshape(bsz, lp, nh, dv)


def mlstm_mixer(q, k, v, o_pre, i_pre, f_pre, i_bias, f_bias, head_norm):
    bsz, n_tok, _ = q.shape
    q = q.astype(jnp.float32).reshape(bsz, n_tok, ML_HEADS, ML_QK_DIM)
    k = k.astype(jnp.float32).reshape(bsz, n_tok, ML_HEADS, ML_QK_DIM) * (ML_QK_DIM ** -0.5)
    v = v.astype(jnp.float32).reshape(bsz, n_tok, ML_HEADS, ML_V_DIM)
    log_i = i_pre.astype(jnp.float32).reshape(bsz, n_tok, 2, ML_HEADS) + i_bias.astype(jnp.float32)
    log_f = jax.nn.log_sigmoid(f_pre.astype(jnp.float32).reshape(bsz, n_tok, 2, ML_HEADS) + f_bias.astype(jnp.float32))
    q_p, k_p, v_p, li_p, lf_p = pad_front(q), pad_front(k), pad_front(v), pad_front(log_i), pad_front(log_f)
    h_f = mlstm_scan(q_p, k_p, v_p, li_p[:, :, 0], lf_p[:, :, 0])
    h_b = flip_seq(mlstm_scan(flip_seq(q_p), flip_seq(k_p), flip_seq(v_p), flip_seq(li_p[:, :, 1]), flip_seq(lf_p[:, :, 1])))
    h = (h_f + h_b)[:, CHUNK - N_META:]
    h = h * lax.rsqrt(jnp.mean(h * h, axis=-1, keepdims=True) + NORM_EPS)
    h = h.reshape(bsz, n_tok, ML_WIDTH) * head_norm.astype(jnp.float32)
    return (jax.nn.sigmoid(o_pre.astype(jnp.float32)) * h).astype(o_pre.dtype)


def fourier_mixer(u):
    bsz, n_tok, _ = u.shape
    uf = u.astype(jnp.float32).reshape(bsz, n_tok, FN_GROUPS, FN_GROUP_DIM)
    y = jnp.real(jnp.fft.fft2(uf, axes=(1, 3), norm='ortho'))
    return y.reshape(bsz, n_tok, FN_WIDTH).astype(u.dtype)


def ab_mixer(u, w_in, conv_w, conv_b, a_log, dt_bias, d_skip, ssd_norm, sink, w_out):
    bsz, n_tok, _ = u.shape
    z, xbc, dt_raw, q, k, v = split_cols(u @ w_in, AB_IN_SIZES)
    y_ssd = ssd_mixer(z, xbc, dt_raw.reshape(bsz, n_tok, 2, SSD_HEADS), conv_w, conv_b, a_log, dt_bias, d_skip, ssd_norm)
    y_att = window_attention(q.reshape(bsz, n_tok, ATT_Q_HEADS, ATT_HEAD_DIM),
                             k.reshape(bsz, n_tok, ATT_KV_HEADS, ATT_HEAD_DIM),
                             v.reshape(bsz, n_tok, ATT_KV_HEADS, ATT_HEAD_DIM), sink)
    return jnp.concatenate([y_ssd, y_att.astype(u.dtype)], axis=-1) @ w_out


def cd_mixer(u, w_in, i_bias, f_bias, head_norm, w_out):
    q, k, v, o_pre, i_pre, f_pre, u_fn = split_cols(u @ w_in, CD_IN_SIZES)
    y_ml = mlstm_mixer(q, k, v, o_pre, i_pre, f_pre, i_bias, f_bias, head_norm)
    y_fn = fourier_mixer(u_fn)
    return jnp.concatenate([y_ml, y_fn], axis=-1) @ w_out


def expert_choice_ffn(u, w_router, w_gate, w_up, w_down):
    bsz, n_tok, d = u.shape
    cap = EC_FACTOR * n_tok // N_EXPERTS
    aff = jax.nn.softmax((u @ w_router).astype(jnp.float32), axis=-1)
    gate, idx = lax.top_k(aff.transpose(0, 2, 1), cap)
    xs = jax.vmap(lambda xb, ib: xb[ib])(u, idx)
    hdn = jax.nn.silu(jnp.einsum('becd,edf->becf', xs, w_gate)) * jnp.einsum('becd,edf->becf', xs, w_up)
    out = jnp.einsum('becf,efd->becd', hdn, w_down)
    out = out * gate[..., None].astype(out.dtype)
    y = jax.vmap(lambda ib, ob: jnp.zeros((n_tok, d), ob.dtype).at[ib].add(ob))(idx, out)
    return y.astype(u.dtype)


def setup_inputs(seed: int = 0) -> dict:
    key = jax.random.key(seed)
    ks = jax.random.split(key, 24)
    n_even = (DEPTH + 1) // 2
    n_odd = DEPTH // 2
    f32 = jnp.float32

    def nrm(k, shape, scale):
        return jax.random.normal(k, shape, f32) * scale

    dt0 = jnp.exp(jax.random.uniform(ks[8], (n_even, 2, SSD_HEADS), f32, math.log(1e-3), math.log(1e-1)))
    return {
        'x': nrm(ks[0], (BATCH, SEQ, D_MODEL), 1.0),
        'meta_tokens': nrm(ks[1], (N_META, D_MODEL), 1.0),
        'norm_mix': 1.0 + nrm(ks[2], (DEPTH, D_MODEL), 0.02),
        'ab_w_in': nrm(ks[3], (n_even, D_MODEL, AB_IN_WIDTH), D_MODEL ** -0.5),
        'ab_conv_w': nrm(ks[4], (n_even, SSD_CONV, SSD_CONV_CH), SSD_CONV ** -0.5),
        'ab_conv_b': nrm(ks[5], (n_even, SSD_CONV_CH), 0.02),
        'ab_a_log': jnp.log(jax.random.uniform(ks[6], (n_even, 2, SSD_HEADS), f32, 1.0, 16.0)),
        'ab_dt_bias': dt0 + jnp.log(-jnp.expm1(-dt0)),
        'ab_d_skip': 1.0 + nrm(ks[7], (n_even, SSD_HEADS), 0.1),
        'ab_ssd_norm': 1.0 + nrm(ks[9], (n_even, SSD_INNER), 0.02),
        'ab_sink': nrm(ks[10], (n_even, ATT_Q_HEADS), 0.5),
        'ab_w_out': nrm(ks[11], (n_even, AB_MIX_WIDTH, D_MODEL), AB_MIX_WIDTH ** -0.5),
        'cd_w_in': nrm(ks[12], (n_odd, D_MODEL, CD_IN_WIDTH), D_MODEL ** -0.5),
        'cd_i_bias': nrm(ks[13], (n_odd, 2, ML_HEADS), 0.1),
        'cd_f_bias': jax.random.uniform(ks[14], (n_odd, 2, ML_HEADS), f32, 3.0, 6.0),
        'cd_head_norm': 1.0 + nrm(ks[15], (n_odd, ML_WIDTH), 0.02),
        'cd_w_out': nrm(ks[16], (n_odd, CD_MIX_WIDTH, D_MODEL), CD_MIX_WIDTH ** -0.5),
        'norm_ffn': 1.0 + nrm(ks[17], (DEPTH, D_MODEL), 0.02),
        'moe_router': nrm(ks[18], (DEPTH, D_MODEL, N_EXPERTS), D_MODEL ** -0.5),
        'moe_w_gate': nrm(ks[19], (DEPTH, N_EXPERTS, D_MODEL, EXPERT_FF), D_MODEL ** -0.5),
        'moe_w_up': nrm(ks[20], (DEPTH, N_EXPERTS, D_MODEL, EXPERT_FF), D_MODEL ** -0.5),
        'moe_w_down': nrm(ks[21], (DEPTH, N_EXPERTS, EXPERT_FF, D_MODEL), EXPERT_FF ** -0.5),
        'final_norm': 1.0 + nrm(ks[22], (D_MODEL,), 0.02),
    }


def reference(x, meta_tokens, norm_mix, ab_w_in, ab_conv_w, ab_conv_b, ab_a_log, ab_dt_bias, ab_d_skip,
              ab_ssd_norm, ab_sink, ab_w_out, cd_w_in, cd_i_bias, cd_f_bias, cd_head_norm, cd_w_out,
              norm_ffn, moe_router, moe_w_gate, moe_w_up, moe_w_down, final_norm):
    bsz = x.shape[0]
    meta = jnp.broadcast_to(meta_tokens.astype(x.dtype)[None], (bsz, N_META, x.shape[-1]))
    h = jnp.concatenate([meta, x], axis=1)
    for layer in range(DEPTH):
        j = layer // 2
        u = rms_norm(h, norm_mix[layer])
        if layer % 2 == 0:
            mix = ab_mixer(u, ab_w_in[j], ab_conv_w[j], ab_conv_b[j], ab_a_log[j], ab_dt_bias[j], ab_d_skip[j],
                           ab_ssd_norm[j], ab_sink[j], ab_w_out[j])
        else:
            mix = cd_mixer(u, cd_w_in[j], cd_i_bias[j], cd_f_bias[j], cd_head_norm[j], cd_w_out[j])
        h = h + mix.astype(h.dtype)
        u = rms_norm(h, norm_ffn[layer])
        h = h + expert_choice_ffn(u, moe_router[layer], moe_w_gate[layer], moe_w_up[layer], moe_w_down[layer])
    return rms_norm(h[:, N_META:], final_norm)
```

```python
import math
from contextlib import ExitStack

import numpy as np
import concourse.bass as bass
import concourse.mybir as mybir
from concourse.bass_utils import run_bass_kernel_spmd

F32 = mybir.dt.float32
BF16 = mybir.dt.bfloat16
AF = mybir.ActivationFunctionType
ALU = mybir.AluOpType
AX = mybir.AxisListType

D = 4096
SEQ = 8192
NMETA = 16
L = SEQ + NMETA
NCORE = 8
TPC = SEQ // NCORE
NT = 1 + TPC // 128
LT = NMETA + TPC
EPS = 1e-6
NEG = -30000.0


def trows(i):
    if i == 0:
        return 0, NMETA
    return NMETA + 128 * (i - 1), 128


class T:
    __slots__ = ("t", "w", "r", "excl", "name")

    def __init__(self, t, name, excl=False):
        self.t = t
        self.w = None
        self.r = []
        self.excl = excl
        self.name = name

    def __getitem__(self, idx):
        return self.t[idx]


class Multi:
    def __init__(self):
        self.tk = {}

    def add(self, ticket):
        k, v = ticket
        if self.tk.get(k, 0) < v:
            self.tk[k] = v


class Prog:
    NDMA = 24

    def __init__(self, nc, stack):
        self.nc = nc
        self.stack = stack
        self.eng_names = ["pe", "dve", "act", "pool", "sp"]
        self.streams = {e: [] for e in self.eng_names}
        self.sems = {}
        for e in self.eng_names:
            self.sems[e] = stack.enter_context(nc.semaphore("s_" + e))
        for k in range(self.NDMA):
            self.sems[("dma", k)] = stack.enter_context(nc.semaphore("s_dma%d" % k))
        self.cnt = {k: 0 for k in self.sems}
        self.known = {e: {} for e in self.eng_names}
        self.dma_rr = 0
        self.n_ins = 0
        self.same_engine_sync = True
        self.rr = {}

    def sb(self, name, shape, dtype):
        t = self.stack.enter_context(self.nc.sbuf_tensor(name, list(shape), dtype))
        return T(t, name)

    def ps(self, name, shape, dtype=F32):
        t = self.stack.enter_context(self.nc.psum_tensor(name, list(shape), dtype))
        return T(t, name, excl=True)

    def din(self, name, shape, dtype=F32):
        return T(self.nc.dram_tensor(name, list(shape), dtype, kind="ExternalInput").ap(), name)

    def dout(self, name, shape, dtype=F32):
        return T(self.nc.dram_tensor(name, list(shape), dtype, kind="ExternalOutput").ap(), name)

    def dscr(self, name, shape, dtype=F32):
        return T(self.nc.dram_tensor(name, list(shape), dtype, kind="Internal").ap(), name)

    def _deps(self, reads, writes):
        deps = []
        for r in reads:
            if r.w is not None:
                deps.append(r.w)
            if r.excl:
                deps.extend(r.r)
        for w in writes:
            if w.w is not None:
                deps.append(w.w)
            deps.extend(w.r)
        return deps

    def _wait(self, eng, deps):
        kn = self.known[eng]
        need = {}
        for (key, val) in deps:
            if key == eng and (eng == "pe" or not self.same_engine_sync):
                continue
            if kn.get(key, 0) >= val:
                continue
            if need.get(key, 0) < val:
                need[key] = val
        for key, val in need.items():
            kn[key] = val
            sem = self.sems[key]
            self.streams[eng].append(lambda e, sem=sem, val=val: e.wait_ge(sem, val))

    def _commit(self, ticket, reads, writes):
        for w in writes:
            w.w = ticket
            w.r = []
        for r in reads:
            if r in writes:
                continue
            r.r.append(ticket)
            if len(r.r) > 64:
                mx = {}
                for k, v in r.r:
                    if mx.get(k, 0) < v:
                        mx[k] = v
                r.r = list(mx.items())

    def op(self, eng, method, reads, writes, *args, after=None, **kwargs):
        reads = list(reads)
        writes = list(writes)
        deps = self._deps(reads, writes)
        if after is not None:
            deps.extend(after.tk.items())
        self._wait(eng, deps)
        self.cnt[eng] += 1
        ticket = (eng, self.cnt[eng])
        sem = self.sems[eng]
        self.streams[eng].append(
            lambda e, m=method, a=args, kw=kwargs, sem=sem: getattr(e, m)(*a, **kw).then_inc(sem, 1))
        self._commit(ticket, reads, writes)
        self.n_ins += 1
        return ticket

    def dma(self, q, out, in_, reads=(), writes=(), multi=None, after=None, **kw):
        reads = list(reads)
        writes = list(writes)
        k = self.dma_rr
        self.dma_rr = (self.dma_rr + 1) % self.NDMA
        key = ("dma", k)
        deps = self._deps(reads, writes)
        if after is not None:
            deps.extend(after.tk.items())
        if self.cnt[key] > 0:
            deps.append((key, self.cnt[key]))
        self._wait(q, deps)
        self.cnt[key] += 16
        ticket = (key, self.cnt[key])
        sem = self.sems[key]
        self.streams[q].append(
            lambda e, out=out, in_=in_, sem=sem, kw=kw: e.dma_start(out=out, in_=in_, **kw).then_inc(sem, 16))
        self._commit(ticket, reads, writes)
        if multi is not None:
            multi.add(ticket)
        self.n_ins += 1
        return ticket

    def idma(self, q, reads=(), writes=(), multi=None, after=None, **kw):
        reads = list(reads)
        writes = list(writes)
        k = self.dma_rr
        self.dma_rr = (self.dma_rr + 1) % self.NDMA
        key = ("dma", k)
        deps = self._deps(reads, writes)
        if after is not None:
            deps.extend(after.tk.items())
        if self.cnt[key] > 0:
            deps.append((key, self.cnt[key]))
        self._wait(q, deps)
        self.cnt[key] += 16
        ticket = (key, self.cnt[key])
        sem = self.sems[key]
        self.streams[q].append(lambda e, sem=sem, kw=kw: e.indirect_dma_start(**kw).then_inc(sem, 16))
        self._commit(ticket, reads, writes)
        if multi is not None:
            multi.add(ticket)
        self.n_ins += 1
        return ticket

    def barrier(self):
        allt = [(k, v) for k, v in self.cnt.items() if v > 0]
        for e in self.eng_names:
            self._wait(e, allt)

    class _Scope:
        def __init__(self, P):
            self.P = P

        def __enter__(self):
            self.old = self.P.stack
            self.es = ExitStack()
            self.es.__enter__()
            self.P.stack = self.es
            return self

        def __exit__(self, *a):
            self.P.barrier()
            self.P.stack = self.old
            return self.es.__exit__(*a)

    def scope(self):
        return Prog._Scope(self)

    def wait_multi(self, eng, multi):
        self._wait(eng, list(multi.tk.items()))

    def pick(self, name, lst):
        i = self.rr.get(name, 0)
        self.rr[name] = i + 1
        return lst[i % len(lst)]

    def emit(self):
        nc = self.nc
        with nc.Block() as block:
            @block.tensor
            def _(e):
                for f in self.streams["pe"]:
                    f(e)

            @block.vector
            def _(e):
                for f in self.streams["dve"]:
                    f(e)

            @block.scalar
            def _(e):
                for f in self.streams["act"]:
                    f(e)

            @block.gpsimd
            def _(e):
                for f in self.streams["pool"]:
                    f(e)

            @block.sync
            def _(e):
                for f in self.streams["sp"]:
                    f(e)


class Ctx:
    pass


def setup_common(P, n_psum=7):
    C = Ctx()
    C.identf = P.sb("identf", [128, 128], F32)
    C.identb = P.sb("identb", [128, 128], BF16)
    P.op("pool", "memset", [], [C.identf], C.identf[:], 0.0)
    P.op("pool", "affine_select", [C.identf], [C.identf], out=C.identf[:], in_=C.identf[:],
         compare_op=ALU.not_equal, fill=1.0, base=0, pattern=[[-1, 128]], channel_multiplier=1)
    P.op("dve", "tensor_copy", [C.identf], [C.identb], C.identb[:], C.identf[:])
    C.ps = [P.ps("ps%d" % i, [128, 512], F32) for i in range(n_psum)]
    return C


def copy_op(P, eng, out_ap, in_ap, reads, writes):
    if eng == "act":
        return P.op("act", "copy", reads, writes, out_ap, in_ap)
    return P.op(eng, "tensor_copy", reads, writes, out_ap, in_ap)


def rstd_from_ss(P, st, col_ss, col_out, nr, n):
    o = st[:nr, col_out:col_out + 1]
    P.op("dve", "tensor_scalar", [st], [st], out=o, in0=st[:nr, col_ss:col_ss + 1],
         scalar1=1.0 / n, scalar2=EPS, op0=ALU.mult, op1=ALU.add)
    P.op("act", "activation", [st], [st], out=o, in_=o, func=AF.Sqrt)
    P.op("dve", "reciprocal", [st], [st], out=o, in_=o)


def transpose_tile(P, C, src, nr, KT, dstT, dst_idx, extra_f32=None, col0=0):
    for k4 in range(0, KT, 4):
        kk = min(4, KT - k4)
        ps = P.pick("tps", C.ps)
        for k in range(kk):
            c = col0 + (k4 + k) * 128
            P.op("pe", "transpose", [src, C.identf], [ps], ps[:, k * 128:k * 128 + nr], src[:nr, c:c + 128],
                 C.identf[:nr, :nr])
        eng = P.pick("tev", ["act", "dve"])
        out_ap = dstT[:, dst_idx, k4:k4 + kk, :nr]
        in_ap = ps[:, 0:kk * 128].rearrange("p (k n) -> p k n", k=kk)[:, :, :nr]
        copy_op(P, eng, out_ap, in_ap, [ps], [dstT])
        if extra_f32 is not None:
            copy_op(P, "dve" if eng == "act" else "act", extra_f32[:, k4:k4 + kk, :nr], in_ap, [ps], [extra_f32])


def gemm_stream(P, C, lhsT, KT, tiles, Wd, N, consume, wbufs, CW=512):
    ncol = (N + CW - 1) // CW
    for j in range(ncol):
        c0 = j * CW
        cw = min(CW, N - c0)
        wb = P.pick("wb", wbufs)
        P.dma("pool", wb[:, :KT, :cw], Wd[:, c0:c0 + cw].rearrange("(k p) n -> p k n", p=128),
              reads=[Wd], writes=[wb])
        for (i, nr) in tiles:
            ps = P.pick("gps", C.ps)
            for k in range(KT):
                P.op("pe", "matmul", [lhsT, wb], [ps], ps[:nr, :cw], lhsT=lhsT[:, i, k, :nr], rhs=wb[:, k, :cw],
                     start=(k == 0), stop=(k == KT - 1))
            consume(i, nr, c0, cw, ps)


def load_bcast_row(P, q, dst, src_row_ap, srcT):
    P.dma(q, dst[:], src_row_ap.partition_broadcast(128), reads=[srcT], writes=[dst])


def build_k1(nparts, N, matmul=True, final=False):
    nc = bass.Bass("TRN2", target_bir_lowering=False)
    with ExitStack() as st:
        P = Prog(nc, st)
        parts = [P.din("part%d" % i, [LT, D]) for i in range(nparts)]
        g = P.din("g", [1, D])
        if matmul:
            W = P.din("w", [D, N])
            out = P.dout("out", [LT, N])
        if nparts > 1 and not final:
            hout = P.dout("hout", [LT, D])
        if final:
            fout = P.dout("fout", [LT, D])
        C = setup_common(P)
        if matmul:
            hnT = P.sb("hnT", [128, NT, 32, 128], BF16)
        outs = Multi()
        with P.scope():
            gb = P.sb("gb", [128, D], F32)
            load_bcast_row(P, "sp", gb, g[0:1, :], g)
            hs = [P.sb("hs%d" % i, [128, D], F32) for i in range(2)]
            tmp = [P.sb("tmp%d" % i, [128, D], F32) for i in range(3)]
            stt = [P.sb("stt%d" % i, [128, 4], F32) for i in range(2)]
            for i in range(NT):
                r0, nr = trows(i)
                h = hs[i % 2]
                s = stt[i % 2]
                P.dma("sp", h[:nr, :], parts[0][r0:r0 + nr, :], reads=[parts[0]], writes=[h])
                for pi in range(1, nparts):
                    t = P.pick("tmp", tmp)
                    P.dma("sp", t[:nr, :], parts[pi][r0:r0 + nr, :], reads=[parts[pi]], writes=[t])
                    eng = P.pick("addeng", ["dve", "pool"])
                    P.op(eng, "tensor_tensor", [h, t], [h], out=h[:nr, :], in0=h[:nr, :], in1=t[:nr, :], op=ALU.add)
                if nparts > 1 and not final:
                    P.dma("sp", hout[r0:r0 + nr, :], h[:nr, :], reads=[h], multi=outs)
                hn = P.pick("tmp", tmp)
                P.op("act", "activation", [h], [hn, s], out=hn[:nr, :], in_=h[:nr, :], func=AF.Square,
                     accum_out=s[:nr, 0:1])
                rstd_from_ss(P, s, 0, 1, nr, D)
                P.op("dve", "scalar_tensor_tensor", [h, s, gb], [hn], out=hn[:nr, :], in0=h[:nr, :],
                     scalar=s[:nr, 1:2], in1=gb[:nr, :], op0=ALU.mult, op1=ALU.mult)
                if final:
                    P.dma("sp", fout[r0:r0 + nr, :], hn[:nr, :], reads=[hn], multi=outs)
                if matmul:
                    transpose_tile(P, C, hn, nr, 32, hnT, i)
        if matmul:
            wbufs = [P.sb("wb%d" % i, [128, 32, 512], BF16) for i in range(2)]
            obufs = [P.sb("ob%d" % i, [128, 512], F32) for i in range(4)]

            def consume(i, nr, c0, cw, ps):
                ob = P.pick("ob", obufs)
                eng = P.pick("oev", ["act", "dve"])
                copy_op(P, eng, ob[:nr, :cw], ps[:nr, :cw], [ps], [ob])
                r0, _ = trows(i)
                P.dma("sp", out[r0:r0 + nr, c0:c0 + cw], ob[:nr, :cw], reads=[ob], multi=outs)

            gemm_stream(P, C, hnT, 32, [(i, trows(i)[1]) for i in range(NT)], W, N, consume, wbufs)
        P.wait_multi("sp", outs)
        P.emit()
    return nc


NSLOT = 65
LP = NSLOT * 128
SG = 13


def build_k2():
    nc = bass.Bass("TRN2", target_bir_lowering=False)
    with ExitStack() as st:
        P = Prog(nc, st)
        xbc = P.din("xbc", [6 * 128, NSLOT, 132])
        convw = P.din("convw", [6 * 128, 6])
        dtr = P.din("dtr", [128, NSLOT * 8])
        hp = P.din("hp", [1, 16])
        cst = P.din("cst", [128, 4 * 128 + NSLOT])
        yout = P.dout("y", [LP, 512])
        xsout = P.dout("xs", [LP, 512])
        C = setup_common(P, n_psum=7)
        pbf = P.ps("pbf", [128, 1024], BF16)
        outs = Multi()
        cs = P.sb("cs", [128, 4 * 128 + NSLOT], F32)
        P.dma("sp", cs[:], cst[:, :], reads=[cst], writes=[cs])
        TRI = cs[:, 0:128]
        NEGM = cs[:, 128:256]
        ONES = cs[:, 256:384]
        NEGONES = cs[:, 384:512]
        VAL0 = 512
        xsT = P.sb("xsT", [128, 4, LP], BF16)
        BT = P.sb("BT", [128, LP], BF16)
        CT = P.sb("CT", [128, LP], BF16)
        dt_all = P.sb("dt_all", [128, NSLOT, 8], F32)
        a_all = P.sb("a_all", [128, NSLOT, 8], F32)
        with P.scope():
            cw = P.sb("cw", [128, 6, 6], F32)
            for t in range(6):
                P.dma("sp", cw[:, t, :], convw[t * 128:(t + 1) * 128, :], reads=[convw], writes=[cw])
            raws = [P.sb("raw%d" % i, [128, SG, 132], F32) for i in range(2)]
            accs = [P.sb("acc%d" % i, [128, SG, 128], F32) for i in range(2)]
            for t in range(6):
                for sgi in range(NSLOT // SG):
                    raw = P.pick("raw", raws)
                    acc = P.pick("acc", accs)
                    s0 = sgi * SG
                    P.dma("sp", raw[:], xbc[t * 128:(t + 1) * 128, s0:s0 + SG, :], reads=[xbc], writes=[raw])
                    P.op("dve", "tensor_scalar", [raw, cw], [acc], out=acc[:], in0=raw[:, :, 0:128],
                         scalar1=cw[:, t, 0:1], scalar2=cw[:, t, 5:6], op0=ALU.mult, op1=ALU.add)
                    for j in range(1, 5):
                        P.op("dve", "scalar_tensor_tensor", [raw, cw, acc], [acc], out=acc[:], in0=raw[:, :, j:j + 128],
                             scalar=cw[:, t, j:j + 1], in1=acc[:], op0=ALU.mult, op1=ALU.add)
                    if t < 4:
                        dst = xsT[:, t, s0 * 128:(s0 + SG) * 128]
                        dT = xsT
                    elif t == 4:
                        dst = BT[:, s0 * 128:(s0 + SG) * 128]
                        dT = BT
                    else:
                        dst = CT[:, s0 * 128:(s0 + SG) * 128]
                        dT = CT
                    P.op("act", "activation", [acc], [dT], out=dst.rearrange("p (s n) -> p s n", s=SG), in_=acc[:],
                         func=AF.Silu)
            hpb = P.sb("hpb", [128, 16], F32)
            load_bcast_row(P, "sp", hpb, hp[0:1, :], hp)
            xr = P.sb("xr", [128, NSLOT, 8], F32)
            t1 = P.sb("t1", [128, NSLOT, 8], F32)
            P.dma("sp", xr[:].rearrange("p s h -> p (s h)"), dtr[:, :], reads=[dtr], writes=[xr])
            P.op("dve", "tensor_tensor", [xr, hpb], [xr], out=xr[:], in0=xr[:],
                 in1=hpb[:, 0:8].rearrange("p (o h) -> p o h", o=1).to_broadcast([128, NSLOT, 8]), op=ALU.add)
            P.op("dve", "scalar_tensor_tensor", [xr], [t1], out=t1[:], in0=xr[:], scalar=-1.0, in1=xr[:],
                 op0=ALU.mult, op1=ALU.max)
            P.op("act", "activation", [t1], [t1], out=t1[:], in_=t1[:], func=AF.Exp, scale=-1.0)
            P.op("act", "activation", [t1], [t1], out=t1[:], in_=t1[:], func=AF.Ln, bias=1.0)
            P.op("dve", "scalar_tensor_tensor", [xr, t1], [dt_all], out=dt_all[:], in0=xr[:], scalar=0.0, in1=t1[:],
                 op0=ALU.max, op1=ALU.add)
            P.op("dve", "tensor_tensor", [dt_all, cs], [dt_all], out=dt_all[:], in0=dt_all[:],
                 in1=cs[:, VAL0:VAL0 + NSLOT].rearrange("p (s o) -> p s o", o=1).to_broadcast([128, NSLOT, 8]),
                 op=ALU.mult)
            P.op("act", "activation", [hpb], [hpb], out=hpb[:, 8:16], in_=hpb[:, 8:16], func=AF.Exp)
            P.op("dve", "scalar_tensor_tensor", [dt_all, hpb], [a_all], out=a_all[:], in0=dt_all[:], scalar=-1.0,
                 in1=hpb[:, 8:16].rearrange("p (o h) -> p o h", o=1).to_broadcast([128, NSLOT, 8]),
                 op0=ALU.mult, op1=ALU.mult)
        H = P.sb("H", [128, 512], F32)
        Hb = P.sb("Hb", [128, 512], BF16)
        P.op("dve", "memset", [], [H], H[:], 0.0)
        P.op("pool", "memset", [], [Hb], Hb[:], 0.0)
        xBs = [P.sb("xB%d" % i, [128, 640], BF16) for i in range(2)]
        sb16s = [P.sb("sb16_%d" % i, [128, 16], F32) for i in range(2)]
        exs = [P.sb("exs%d" % i, [128, 24], F32) for i in range(2)]
        aTRIs = [P.sb("aTRI%d" % i, [128, 128], F32) for i in range(4)]
        Es = [P.sb("E%d" % i, [128, 4, 128], F32) for i in range(2)]
        MTs = [P.sb("MT%d" % i, [128, 8, 128], BF16) for i in range(2)]
        xdts = [P.sb("xdt%d" % i, [128, 512], BF16) for i in range(2)]
        xdds = [P.sb("xdd%d" % i, [128, 512], BF16) for i in range(2)]
        yoffs = [P.sb("yoff%d" % i, [128, 512], F32) for i in range(2)]
        ysbs = [P.sb("ysb%d" % i, [128, 512], F32) for i in range(2)]
        pa, pD0, pD1, pcb, py, pyo, pS = C.ps
        for k in range(NSLOT):
            t0 = k * 128
            xB = xBs[k % 2]
            sb16 = sb16s[k % 2]
            ex = exs[k % 2]
            MT = MTs[k % 2]
            xdt = xdts[k % 2]
            xdd = xdds[k % 2]
            yoff = yoffs[k % 2]
            ysb = ysbs[k % 2]
            for j in range(4):
                P.op("pe", "transpose", [xsT, C.identb], [pbf], pbf[:, j * 128:(j + 1) * 128], xsT[:, j, t0:t0 + 128],
                     C.identb[:, :])
            P.op("pe", "transpose", [BT, C.identb], [pbf], pbf[:, 512:640], BT[:, t0:t0 + 128], C.identb[:, :])
            P.op("act", "mul", [pbf, cs], [xB], xB[:, :], pbf[:, 0:640], cs[:, VAL0 + k:VAL0 + k + 1])
            P.dma("pool", xsout[t0:t0 + 128, :], xB[:, 0:512], reads=[xB], multi=outs)
            P.op("pe", "matmul", [cs, a_all], [pa], pa[:, 0:8], lhsT=TRI, rhs=a_all[:, k, :], start=True, stop=True)
            P.op("pe", "matmul", [cs, a_all], [pa], pa[:, 8:16], lhsT=ONES, rhs=a_all[:, k, :], start=True, stop=True)
            P.op("act", "copy", [pa], [sb16], sb16[:, :], pa[:, 0:16])
            P.op("act", "activation", [sb16], [ex], out=ex[:, 0:8], in_=sb16[:, 0:8], func=AF.Exp)
            P.op("act", "activation", [sb16], [ex], out=ex[:, 16:24], in_=sb16[:, 8:16], func=AF.Exp)
            P.op("dve", "tensor_tensor", [sb16], [sb16], out=sb16[:, 8:16], in0=sb16[:, 8:16], in1=sb16[:, 0:8],
                 op=ALU.subtract)
            P.op("act", "activation", [sb16], [ex], out=ex[:, 8:16], in_=sb16[:, 8:16], func=AF.Exp)
            P.op("pe", "matmul", [BT, CT], [pcb], pcb[:, 0:128], lhsT=BT[:, t0:t0 + 128], rhs=CT[:, t0:t0 + 128],
                 start=True, stop=True)
            for hg in range(2):
                pD = pD0 if hg == 0 else pD1
                E = Es[hg]
                for hh in range(4):
                    h = hg * 4 + hh
                    aT = P.pick("aTRI", aTRIs)
                    P.op("dve", "tensor_scalar", [cs, a_all], [aT], out=aT[:, :], in0=TRI, scalar1=a_all[:, k, h:h + 1],
                         scalar2=None, op0=ALU.mult)
                    o = pD[:, hh * 128:(hh + 1) * 128]
                    P.op("pe", "matmul", [cs, aT], [pD], o, lhsT=ONES, rhs=aT[:, :], start=True, stop=False,
                         skip_group_check=True)
                    P.op("pe", "matmul", [cs, aT], [pD], o, lhsT=aT[:, :], rhs=NEGONES, start=False, stop=False,
                         skip_group_check=True)
                    P.op("pe", "matmul", [cs, C.identf], [pD], o, lhsT=C.identf[:, :], rhs=NEGM, start=False, stop=True,
                         skip_group_check=True)
                P.op("act", "activation", [pD], [E], out=E[:].rearrange("p h n -> p (h n)"), in_=pD[:, :], func=AF.Exp)
                for hh in range(4):
                    h = hg * 4 + hh
                    P.op("dve", "tensor_tensor", [E, pcb], [MT], out=MT[:, h, :], in0=E[:, hh, :], in1=pcb[:, 0:128],
                         op=ALU.mult)
            dtb = dt_all[:, k, :].rearrange("p (h o) -> p h o", o=1).to_broadcast([128, 8, 64])
            P.op("dve", "tensor_tensor", [xB, dt_all], [xdt], out=xdt[:].rearrange("p (h d) -> p h d", h=8),
                 in0=xB[:, 0:512].rearrange("p (h d) -> p h d", h=8), in1=dtb, op=ALU.mult)
            P.op("dve", "tensor_tensor", [xdt, ex], [xdd], out=xdd[:].rearrange("p (h d) -> p h d", h=8),
                 in0=xdt[:].rearrange("p (h d) -> p h d", h=8),
                 in1=ex[:, 8:16].rearrange("p (h o) -> p h o", o=1).to_broadcast([128, 8, 64]), op=ALU.mult)
            for h in range(8):
                P.op("pe", "matmul", [MT, xdt], [py], py[:, h * 64:(h + 1) * 64], lhsT=MT[:, h, :],
                     rhs=xdt[:, h * 64:(h + 1) * 64], start=True, stop=True, skip_group_check=True)
            P.op("pe", "matmul", [CT, Hb], [pyo], pyo[:, :], lhsT=CT[:, t0:t0 + 128], rhs=Hb[:, :], start=True, stop=True)
            P.op("act", "copy", [pyo], [yoff], yoff[:, :], pyo[:, :])
            P.op("dve", "tensor_tensor", [yoff, ex], [yoff], out=yoff[:].rearrange("p (h d) -> p h d", h=8),
                 in0=yoff[:].rearrange("p (h d) -> p h d", h=8),
                 in1=ex[:, 0:8].rearrange("p (h o) -> p h o", o=1).to_broadcast([128, 8, 64]), op=ALU.mult)
            P.op("dve", "tensor_tensor", [yoff, py], [ysb], out=ysb[:, :], in0=yoff[:, :], in1=py[:, :], op=ALU.add)
            P.dma("sp", yout[t0:t0 + 128, :], ysb[:, :], reads=[ysb], multi=outs)
            P.op("pe", "matmul", [xB, xdd], [pS], pS[:, :], lhsT=xB[:, 512:640], rhs=xdd[:, :], start=True, stop=True)
            P.op("dve", "tensor_tensor", [H, ex], [H], out=H[:].rearrange("p (h d) -> p h d", h=8),
                 in0=H[:].rearrange("p (h d) -> p h d", h=8),
                 in1=ex[:, 16:24].rearrange("p (h o) -> p h o", o=1).to_broadcast([128, 8, 64]), op=ALU.mult)
            P.op("dve", "tensor_tensor", [H, pS], [H], out=H[:, :], in0=H[:, :], in1=pS[:, :], op=ALU.add)
            P.op("act", "copy", [H], [Hb], Hb[:, :], H[:, :])
        P.wait_multi("sp", outs)
        P.emit()
    return nc


def k2_consts(direction):
    k = np.arange(128)[:, None]
    l = np.arange(128)[None, :]
    if direction == 0:
        tri = (k <= l)
        negm = np.where(l >= k, 0.0, NEG)
    else:
        tri = (k >= l)
        negm = np.where(l <= k, 0.0, NEG)
    valid = np.ones((128, NSLOT), np.float32)
    meta_slot = 0 if direction == 0 else NSLOT - 1
    valid[:112, meta_slot] = 0.0
    return np.concatenate([tri.astype(np.float32), negm.astype(np.float32), np.ones((128, 128), np.float32),
                           -np.ones((128, 128), np.float32), valid], axis=1)


def k2_host_inputs(proj, conv_w, conv_b, a_log, dt_bias, c):
    g, d = c // 2, c % 2
    xbc = proj[:, 2048:5120]
    cols = np.concatenate([np.arange(g * 512, (g + 1) * 512), 2048 + np.arange(g * 128, (g + 1) * 128),
                           2560 + np.arange(g * 128, (g + 1) * 128)])
    full = np.zeros((LP + 4, 768), np.float32)
    full[2 + 112:2 + 112 + L] = xbc[:, cols]
    order = np.arange(NSLOT) if d == 0 else np.arange(NSLOT)[::-1]
    idx = order[:, None] * 128 + np.arange(132)[None, :]
    blk = full[idx]
    xbc_blk = np.ascontiguousarray(blk.transpose(2, 0, 1))
    cw = np.concatenate([conv_w[:, 2048 * 0:][:, cols].T, conv_b[cols][:, None]], axis=1)
    dtraw = proj[:, 5120:5184].reshape(L, 2, 32)[:, d, g * 8:(g + 1) * 8]
    dfull = np.zeros((LP, 8), np.float32)
    dfull[112:] = dtraw
    dtr = dfull.reshape(NSLOT, 128, 8)[order].transpose(1, 0, 2).reshape(128, NSLOT * 8)
    hp = np.concatenate([dt_bias[d, g * 8:(g + 1) * 8], a_log[d, g * 8:(g + 1) * 8]])[None, :]
    return {"xbc": xbc_blk, "convw": np.ascontiguousarray(cw, dtype=np.float32), "dtr": np.ascontiguousarray(dtr),
            "hp": np.ascontiguousarray(hp, dtype=np.float32), "cst": k2_consts(d)}


def k2_unslot(y, d):
    order = np.arange(NSLOT) if d == 0 else np.arange(NSLOT)[::-1]
    out = np.empty_like(y).reshape(NSLOT, 128, -1)
    out[order] = y.reshape(NSLOT, 128, -1)
    return out.reshape(LP, -1)[112:]


NKB = 10
ATT_SCALE = 128 ** -0.5


def build_k3():
    nc = bass.Bass("TRN2", target_bir_lowering=False)
    with ExitStack() as st:
        P = Prog(nc, st)
        qT = P.din("qT", [16 * 128, LT])
        kT = P.din("kT", [4 * 128, 16 + NKB * 128])
        va = P.din("va", [16 + NKB * 128, 4 * 129])
        kTm = P.din("kTm", [4 * 128, 144])
        vam = P.din("vam", [144, 4 * 129])
        masks = P.din("masks", [128, 16 * 384])
        maskm = P.din("maskm", [128, 32])
        sink = P.din("sink", [1, 16])
        yatt = P.dout("yatt", [LT, 2048])
        C = setup_common(P, n_psum=8)
        outs = Multi()
        q_sb = P.sb("q_sb", [128, 16, LT], BF16)
        k_sb = P.sb("k_sb", [128, 4, 16 + NKB * 128], BF16)
        v_sb = P.sb("v_sb", [128, NKB, 516], BF16)
        vm_sb = P.sb("vm_sb", [16, 516], BF16)
        km_sb = P.sb("km_sb", [128, 4, 144], BF16)
        vmm = P.sb("vmm", [128, 2, 516], BF16)
        mk = P.sb("mk", [128, 16, 384], F32)
        mkm = P.sb("mkm", [128, 32], F32)
        esink = P.sb("esink", [128, 16], F32)
        P.dma("pool", q_sb[:], qT[:, :].rearrange("(h p) n -> p h n", p=128), reads=[qT], writes=[q_sb])
        P.dma("pool", k_sb[:], kT[:, :].rearrange("(h p) n -> p h n", p=128), reads=[kT], writes=[k_sb])
        P.dma("pool", v_sb[:], va[16:, :].rearrange("(b p) n -> p b n", p=128), reads=[va], writes=[v_sb])
        P.dma("pool", vm_sb[:], va[0:16, :], reads=[va], writes=[vm_sb])
        P.dma("pool", km_sb[:], kTm[:, :].rearrange("(h p) n -> p h n", p=128), reads=[kTm], writes=[km_sb])
        P.dma("pool", vmm[:, 0, :], vam[0:128, :], reads=[vam], writes=[vmm])
        P.dma("pool", vmm[:16, 1, :], vam[128:144, :], reads=[vam], writes=[vmm])
        P.dma("sp", mk[:].rearrange("p h n -> p (h n)"), masks[:, :], reads=[masks], writes=[mk])
        P.dma("sp", mkm[:], maskm[:, :], reads=[maskm], writes=[mkm])
        load_bcast_row(P, "sp", esink, sink[0:1, :], sink)
        P.op("act", "activation", [esink], [esink], out=esink[:], in_=esink[:], func=AF.Exp)
        scs = [P.sb("sc%d" % i, [128, 384], F32) for i in range(2)]
        PTs = [P.sb("PT%d" % i, [128, 512], BF16) for i in range(2)]
        dens = [P.sb("den%d" % i, [128, 2], F32) for i in range(2)]
        ytile = [P.sb("ytile%d" % i, [128, 2048], F32) for i in range(2)]
        pS = C.ps[0:4]
        pO = C.ps[4:8]
        for i in range(NT):
            r0, nr = trows(i)
            yt = ytile[i % 2]
            for h in range(16):
                kvh = h // 4
                ps = P.pick("pS", pS)
                po = P.pick("pO", pO)
                sc = P.pick("sc", scs)
                PT = P.pick("PT", PTs)
                den = P.pick("den", dens)
                if i == 0:
                    q = q_sb[:, h, 0:16]
                    P.op("pe", "matmul", [km_sb, q_sb], [ps], ps[:, 0:16], lhsT=km_sb[:, kvh, 0:128], rhs=q,
                         start=True, stop=True, skip_group_check=True)
                    P.op("pe", "matmul", [km_sb, q_sb], [ps], ps[:16, 16:32], lhsT=km_sb[:, kvh, 128:144], rhs=q,
                         start=True, stop=True, skip_group_check=True)
                    P.op("dve", "scalar_tensor_tensor", [ps, mkm], [sc], out=sc[:, 0:32], in0=ps[:, 0:32],
                         scalar=ATT_SCALE, in1=mkm[:, 0:32], op0=ALU.mult, op1=ALU.add)
                    P.op("act", "activation", [sc], [PT], out=PT[:, 0:32], in_=sc[:, 0:32], func=AF.Exp)
                    P.op("pe", "matmul", [PT, vmm], [po], po[:16, 0:129], lhsT=PT[:, 0:16],
                         rhs=vmm[:, 0, kvh * 129:(kvh + 1) * 129], start=True, stop=False)
                    P.op("pe", "matmul", [PT, vmm], [po], po[:16, 0:129], lhsT=PT[:16, 16:32],
                         rhs=vmm[:16, 1, kvh * 129:(kvh + 1) * 129], start=False, stop=True)
                else:
                    b = i - 1
                    q = q_sb[:, h, r0:r0 + 128]
                    for sgm in range(3):
                        kc = 16 + (b + sgm) * 128
                        P.op("pe", "matmul", [k_sb, q_sb], [ps], ps[:, sgm * 128:(sgm + 1) * 128],
                             lhsT=k_sb[:, kvh, kc:kc + 128], rhs=q, start=True, stop=True, skip_group_check=True)
                    P.op("pe", "matmul", [k_sb, q_sb], [ps], ps[:16, 384:512], lhsT=k_sb[:, kvh, 0:16], rhs=q,
                         start=True, stop=True, skip_group_check=True)
                    P.op("dve", "scalar_tensor_tensor", [ps, mk], [sc], out=sc[:, :], in0=ps[:, 0:384],
                         scalar=ATT_SCALE, in1=mk[:, h, :], op0=ALU.mult, op1=ALU.add)
                    P.op("act", "activation", [sc], [PT], out=PT[:, 0:384], in_=sc[:, :], func=AF.Exp)
                    P.op("act", "activation", [ps], [PT], out=PT[:16, 384:512], in_=ps[:16, 384:512], func=AF.Exp,
                         scale=ATT_SCALE)
                    for sgm in range(3):
                        P.op("pe", "matmul", [PT, v_sb], [po], po[:, 0:129], lhsT=PT[:, sgm * 128:(sgm + 1) * 128],
                             rhs=v_sb[:, b + sgm, kvh * 129:(kvh + 1) * 129], start=(sgm == 0), stop=False)
                    P.op("pe", "matmul", [PT, vm_sb], [po], po[:, 0:129], lhsT=PT[:16, 384:512],
                         rhs=vm_sb[:16, kvh * 129:(kvh + 1) * 129], start=False, stop=True)
                P.op("dve", "tensor_tensor", [po, esink], [den], out=den[:nr, 0:1], in0=po[:nr, 128:129],
                     in1=esink[:nr, h:h + 1], op=ALU.add)
                P.op("dve", "reciprocal", [den], [den], out=den[:nr, 1:2], in_=den[:nr, 0:1])
                P.op("act", "mul", [po, den], [yt], yt[:nr, h * 128:(h + 1) * 128], po[:nr, 0:128], den[:nr, 1:2])
            P.dma("sp", yatt[r0:r0 + nr, :], yt[:nr, :], reads=[yt], multi=outs)
        P.wait_multi("sp", outs)
        P.emit()
    return nc


def k3_consts():
    slopes = 2.0 ** (-8.0 * np.arange(1, 17, dtype=np.float64) / 16)
    k = np.arange(128)[:, None]
    q = np.arange(128)[None, :]
    m = np.zeros((128, 16, 3, 128), np.float32)
    for h in range(16):
        dist = q + 128 - k
        m[:, h, 0, :] = np.where(dist <= 128, -slopes[h] * dist, NEG)
        dist = np.abs(k - q)
        m[:, h, 1, :] = -slopes[h] * dist
        dist = k + 128 - q
        m[:, h, 2, :] = np.where(dist <= 128, -slopes[h] * dist, NEG)
    mm = np.zeros((128, 32), np.float32)
    qi = np.arange(16)[None, :]
    mm[:, 0:16] = np.where(np.abs(np.arange(128)[:, None] - qi) <= 128, 0.0, NEG)
    mm[:16, 16:32] = np.where(np.abs(128 + np.arange(16)[:, None] - qi) <= 128, 0.0, NEG)
    return m.reshape(128, 16 * 384), mm


def k3_host_inputs(proj, sink, c, consts):
    q = proj[:, 5184:7232]
    kk = proj[:, 7232:7744]
    v = proj[:, 7744:8256]
    rows = np.concatenate([np.arange(16), 16 + c * TPC + np.arange(TPC)])
    qT = np.ascontiguousarray(q[rows].T)
    nk = 16 + NKB * 128
    kpad = np.zeros((nk, 512), np.float32)
    vpad = np.zeros((nk, 4, 129), np.float32)
    kpad[:16] = kk[:16]
    vpad[:16, :, :128] = v[:16].reshape(16, 4, 128)
    vpad[:16, :, 128] = 1.0
    for j in range(NKB):
        blk = c * 8 - 1 + j
        if 0 <= blk < SEQ // 128:
            t0 = 16 + blk * 128
            kpad[16 + j * 128:16 + (j + 1) * 128] = kk[t0:t0 + 128]
            vpad[16 + j * 128:16 + (j + 1) * 128, :, :128] = v[t0:t0 + 128].reshape(128, 4, 128)
            vpad[16 + j * 128:16 + (j + 1) * 128, :, 128] = 1.0
    kTm = np.ascontiguousarray(kk[:144].T)
    vam = np.ones((144, 4, 129), np.float32)
    vam[:, :, :128] = v[:144].reshape(144, 4, 128)
    return {"qT": qT, "kT": np.ascontiguousarray(kpad.T), "va": vpad.reshape(nk, 516), "kTm": kTm,
            "vam": vam.reshape(144, 516), "masks": consts[0], "maskm": consts[1],
            "sink": np.ascontiguousarray(sink[None, :], dtype=np.float32)}


def build_k4(layer):
    nc = bass.Bass("TRN2", target_bir_lowering=False)
    with ExitStack() as st:
        P = Prog(nc, st)
        if layer == 0:
            yf = P.din("yf", [LT, 2048])
            yb = P.din("yb", [LT, 2048])
            xs = P.din("xs", [LT, 2048])
            z = P.din("z", [LT, 2048])
            yatt = P.din("yatt", [LT, 2048])
            vec = P.din("vec", [1, 4096])
        else:
            hf = P.din("hf", [LT, 3072])
            hb = P.din("hb", [LT, 3072])
            opre = P.din("opre", [LT, 3072])
            yfn = P.din("yfn", [LT, 1024])
            vec = P.din("vec", [1, 3072])
        hin = P.din("h", [LT, D])
        g2 = P.din("g2", [1, D])
        Wout = P.din("wout", [D, D])
        Wr = P.din("wr", [D, 16])
        h1 = P.dout("h1", [LT, D])
        u2T = P.dout("u2T", [D, LT], BF16)
        aff = P.dout("aff", [LT, 16])
        C = setup_common(P)
        ycT = P.sb("ycT", [128, NT, 32, 128], BF16)
        outs = Multi()
        h1w = Multi()
        with P.scope():
            vb = P.sb("vb", [128, vec.t.shape[1]], F32)
            load_bcast_row(P, "sp", vb, vec[0:1, :], vec)
            ycs = [P.sb("yc%d" % i, [128, D], F32) for i in range(2)]
            ta = [P.sb("ta%d" % i, [128, 3072], F32) for i in range(2)]
            tb = [P.sb("tb%d" % i, [128, 3072], F32) for i in range(2)]
            stt = [P.sb("stt%d" % i, [128, 16], F32) for i in range(2)]
            for i in range(NT):
                r0, nr = trows(i)
                yc = ycs[i % 2]
                a = ta[i % 2]
                b = tb[i % 2]
                s = stt[i % 2]
                if layer == 0:
                    P.dma("sp", a[:nr, 0:2048], yf[r0:r0 + nr, :], reads=[yf], writes=[a])
                    P.dma("sp", b[:nr, 0:2048], yb[r0:r0 + nr, :], reads=[yb], writes=[b])
                    P.op("dve", "tensor_tensor", [a, b], [a], out=a[:nr, 0:2048], in0=a[:nr, 0:2048], in1=b[:nr, 0:2048],
                         op=ALU.add)
                    P.dma("sp", b[:nr, 0:2048], xs[r0:r0 + nr, :], reads=[xs], writes=[b])
                    P.op("pool", "tensor_tensor", [b, vb], [b], out=b[:nr, 0:2048], in0=b[:nr, 0:2048],
                         in1=vb[:nr, 0:2048], op=ALU.mult)
                    P.op("dve", "tensor_tensor", [a, b], [a], out=a[:nr, 0:2048], in0=a[:nr, 0:2048], in1=b[:nr, 0:2048],
                         op=ALU.add)
                    P.dma("sp", b[:nr, 0:2048], z[r0:r0 + nr, :], reads=[z], writes=[b])
                    P.op("act", "activation", [b], [b], out=b[:nr, 0:2048], in_=b[:nr, 0:2048], func=AF.Silu)
                    P.op("dve", "tensor_tensor", [a, b], [a], out=a[:nr, 0:2048], in0=a[:nr, 0:2048], in1=b[:nr, 0:2048],
                         op=ALU.mult)
                    for gidx in range(4):
                        sl = slice(gidx * 512, (gidx + 1) * 512)
                        P.op("act", "activation", [a], [b, s], out=b[:nr, sl], in_=a[:nr, sl], func=AF.Square,
                             accum_out=s[:nr, gidx:gidx + 1])
                    for gidx in range(4):
                        rstd_from_ss(P, s, gidx, 8 + gidx, nr, 512)
                    for gidx in range(4):
                        sl = slice(gidx * 512, (gidx + 1) * 512)
                        P.op("dve", "scalar_tensor_tensor", [a, s, vb], [yc], out=yc[:nr, sl], in0=a[:nr, sl],
                             scalar=s[:nr, 8 + gidx:9 + gidx], in1=vb[:nr, 2048 + gidx * 512:2048 + (gidx + 1) * 512],
                             op0=ALU.mult, op1=ALU.mult)
                    P.dma("sp", yc[:nr, 2048:4096], yatt[r0:r0 + nr, :], reads=[yatt], writes=[yc])
                else:
                    P.dma("sp", a[:nr, :], hf[r0:r0 + nr, :], reads=[hf], writes=[a])
                    P.dma("sp", b[:nr, :], hb[r0:r0 + nr, :], reads=[hb], writes=[b])
                    P.op("dve", "tensor_tensor", [a, b], [a], out=a[:nr, :], in0=a[:nr, :], in1=b[:nr, :], op=ALU.add)
                    for hd in range(6):
                        sl = slice(hd * 512, (hd + 1) * 512)
                        P.op("act", "activation", [a], [b, s], out=b[:nr, sl], in_=a[:nr, sl], func=AF.Square,
                             accum_out=s[:nr, hd:hd + 1])
                    for hd in range(6):
                        rstd_from_ss(P, s, hd, 8 + hd, nr, 512)
                    P.dma("sp", b[:nr, :], opre[r0:r0 + nr, :], reads=[opre], writes=[b])
                    P.op("act", "activation", [b], [b], out=b[:nr, :], in_=b[:nr, :], func=AF.Sigmoid)
                    for hd in range(6):
                        sl = slice(hd * 512, (hd + 1) * 512)
                        P.op("dve", "scalar_tensor_tensor", [a, s, vb], [a], out=a[:nr, sl], in0=a[:nr, sl],
                             scalar=s[:nr, 8 + hd:9 + hd], in1=vb[:nr, sl], op0=ALU.mult, op1=ALU.mult)
                    P.op("pool", "tensor_tensor", [a, b], [yc], out=yc[:nr, 0:3072], in0=a[:nr, :], in1=b[:nr, :],
                         op=ALU.mult)
                    P.dma("sp", yc[:nr, 3072:4096], yfn[r0:r0 + nr, :], reads=[yfn], writes=[yc])
                transpose_tile(P, C, yc, nr, 32, ycT, i)
        with P.scope():
            wbufs = [P.sb("wb%d" % i, [128, 32, 512], BF16) for i in range(2)]
            hbufs = [P.sb("hb%d" % i, [128, 512], F32) for i in range(4)]

            def consume(i, nr, c0, cw, ps):
                r0, _ = trows(i)
                hb_ = P.pick("hb", hbufs)
                P.dma("sp", hb_[:nr, :cw], hin[r0:r0 + nr, c0:c0 + cw], reads=[hin], writes=[hb_])
                P.op("dve", "tensor_tensor", [hb_, ps], [hb_], out=hb_[:nr, :cw], in0=hb_[:nr, :cw], in1=ps[:nr, :cw],
                     op=ALU.add)
                P.dma("sp", h1[r0:r0 + nr, c0:c0 + cw], hb_[:nr, :cw], reads=[hb_], multi=h1w)

            gemm_stream(P, C, ycT, 32, [(i, trows(i)[1]) for i in range(NT)], Wout, D, consume, wbufs)
        gb = P.sb("gb", [128, D], F32)
        load_bcast_row(P, "sp", gb, g2[0:1, :], g2)
        wr = P.sb("wr_sb", [128, 32, 16], F32)
        P.dma("sp", wr[:], Wr[:, :].rearrange("(k p) n -> p k n", p=128), reads=[Wr], writes=[wr])
        hs = [P.sb("hs%d" % i, [128, D], F32) for i in range(2)]
        hns = [P.sb("hn%d" % i, [128, D], F32) for i in range(2)]
        uT = [P.sb("uT%d" % i, [128, 1, 32, 128], BF16) for i in range(2)]
        uTf = [P.sb("uTf%d" % i, [128, 32, 128], F32) for i in range(2)]
        stt = [P.sb("st3_%d" % i, [128, 8], F32) for i in range(2)]
        afs = [P.sb("af%d" % i, [128, 16], F32) for i in range(2)]
        for i in range(NT):
            r0, nr = trows(i)
            h = hs[i % 2]
            hn = hns[i % 2]
            s = stt[i % 2]
            P.dma("sp", h[:nr, :], h1[r0:r0 + nr, :], reads=[h1], writes=[h], after=h1w)
            P.op("act", "activation", [h], [hn, s], out=hn[:nr, :], in_=h[:nr, :], func=AF.Square, accum_out=s[:nr, 0:1])
            rstd_from_ss(P, s, 0, 1, nr, D)
            P.op("dve", "scalar_tensor_tensor", [h, s, gb], [hn], out=hn[:nr, :], in0=h[:nr, :], scalar=s[:nr, 1:2],
                 in1=gb[:nr, :], op0=ALU.mult, op1=ALU.mult)
            ut = uT[i % 2]
            utf = uTf[i % 2]
            transpose_tile(P, C, hn, nr, 32, ut, 0, extra_f32=utf)
            P.dma("sp", u2T[:, r0:r0 + nr].rearrange("(k p) n -> p k n", p=128), ut[:, 0, :, :nr], reads=[ut], multi=outs)
            pr = P.pick("gps", C.ps)
            for k in range(32):
                P.op("pe", "matmul", [utf, wr], [pr], pr[:nr, 0:16], lhsT=utf[:, k, :nr], rhs=wr[:, k, :],
                     start=(k == 0), stop=(k == 31))
            af = afs[i % 2]
            P.op("dve", "reduce_max", [pr], [s], out=s[:nr, 2:3], in_=pr[:nr, 0:16], axis=AX.X)
            P.op("dve", "tensor_scalar", [s], [s], out=s[:nr, 3:4], in0=s[:nr, 2:3], scalar1=-1.0, scalar2=None,
                 op0=ALU.mult)
            P.op("act", "activation", [pr, s], [af, s], out=af[:nr, :], in_=pr[:nr, 0:16], func=AF.Exp, bias=s[:nr, 3:4],
                 accum_out=s[:nr, 4:5])
            P.op("dve", "reciprocal", [s], [s], out=s[:nr, 5:6], in_=s[:nr, 4:5])
            P.op("dve", "tensor_scalar", [af, s], [af], out=af[:nr, :], in0=af[:nr, :], scalar1=s[:nr, 5:6], scalar2=None,
                 op0=ALU.mult)
            P.dma("sp", aff[r0:r0 + nr, :], af[:nr, :], reads=[af], multi=outs)
        P.wait_multi("sp", outs)
        P.wait_multi("sp", h1w)
        P.emit()
    return nc


CAP = 2 * L // 16
FF = 1536
TG = 512


def build_k7():
    nc = bass.Bass("TRN2", target_bir_lowering=False)
    with ExitStack() as st:
        P = Prog(nc, st)
        u2T = P.din("u2T", [D, L], BF16)
        affT = P.din("affT", [2, L])
        wg = P.din("wg", [2 * D, FF])
        wu = P.din("wu", [2 * D, FF])
        wd = P.din("wd", [2 * FF, D])
        y = P.dout("y", [L, D])
        C = setup_common(P, n_psum=8)
        outs = Multi()
        thr = P.sb("thr", [128, 2], F32)
        with P.scope():
            arow = P.sb("arow", [128, L], F32)
            junk = P.sb("junk", [128, L], BF16)
            b = P.sb("bis", [128, 8], F32)
            for e in range(2):
                P.dma("sp", arow[:], affT[e:e + 1, :].partition_broadcast(128), reads=[affT], writes=[arow])
                P.op("dve", "memset", [], [b], b[:, 0:1], 0.0)
                P.op("dve", "memset", [], [b], b[:, 1:2], 1.0)
                for it in range(32):
                    P.op("dve", "tensor_tensor", [b], [b], out=b[:, 2:3], in0=b[:, 0:1], in1=b[:, 1:2], op=ALU.add)
                    P.op("dve", "tensor_scalar", [b], [b], out=b[:, 2:3], in0=b[:, 2:3], scalar1=0.5, scalar2=None,
                         op0=ALU.mult)
                    P.op("dve", "memset", [], [b], b[:, 3:4], 0.0)
                    P.op("dve", "tensor_scalar", [arow, b], [junk, b], out=junk[:], in0=arow[:], scalar1=b[:, 2:3],
                         scalar2=0.0, op0=ALU.is_ge, op1=ALU.add, accum_out=b[:, 3:4])
                    P.op("dve", "tensor_scalar", [b], [b], out=b[:, 4:5], in0=b[:, 3:4], scalar1=float(CAP) - 0.5,
                         scalar2=None, op0=ALU.is_ge)
                    P.op("dve", "tensor_scalar", [b], [b], out=b[:, 6:7], in0=b[:, 4:5], scalar1=-1.0, scalar2=1.0,
                         op0=ALU.mult, op1=ALU.add)
                    P.op("dve", "tensor_tensor", [b], [b], out=b[:, 5:6], in0=b[:, 2:3], in1=b[:, 0:1], op=ALU.subtract)
                    P.op("dve", "scalar_tensor_tensor", [b], [b], out=b[:, 0:1], in0=b[:, 5:6], scalar=b[:, 4:5],
                         in1=b[:, 0:1], op0=ALU.mult, op1=ALU.add)
                    P.op("dve", "tensor_tensor", [b], [b], out=b[:, 5:6], in0=b[:, 2:3], in1=b[:, 1:2], op=ALU.subtract)
                    P.op("dve", "scalar_tensor_tensor", [b], [b], out=b[:, 1:2], in0=b[:, 5:6], scalar=b[:, 6:7],
                         in1=b[:, 1:2], op0=ALU.mult, op1=ALU.add)
                P.op("dve", "tensor_copy", [b], [thr], thr[:, e:e + 1], b[:, 0:1])
        ug = P.sb("ug", [128, 32, TG], BF16)
        hdnT = P.sb("hdnT", [128, 24, TG], BF16)
        wgb = [P.sb("wgb%d" % i, [128, 32, 256], BF16) for i in range(2)]
        wub = [P.sb("wub%d" % i, [128, 32, 256], BF16) for i in range(2)]
        wdb = [P.sb("wdb%d" % i, [128, 24, 512], BF16) for i in range(2)]
        gms = [P.sb("gm%d" % i, [128, 2, TG], F32) for i in range(2)]
        sgs = [P.sb("sg%d" % i, [128, TG], F32) for i in range(2)]
        obufs = [P.sb("ob%d" % i, [128, 512], F32) for i in range(4)]
        psg = C.ps[0:2]
        psu = C.ps[2:4]
        psd = C.ps[4:8]
        ngroups = (L + TG - 1) // TG
        for gi in range(ngroups):
            t0 = gi * TG
            n = min(TG, L - t0)
            gm = gms[gi % 2]
            P.dma("sp", ug[:, :, :n], u2T[:, t0:t0 + n].rearrange("(k p) n -> p k n", p=128), reads=[u2T], writes=[ug])
            for e in range(2):
                P.dma("sp", gm[:, e, :n], affT[e:e + 1, t0:t0 + n].partition_broadcast(128), reads=[affT], writes=[gm])
            for e in range(2):
                P.op("dve", "scalar_tensor_tensor", [gm, thr], [gm], out=gm[:, e, :n], in0=gm[:, e, :n],
                     scalar=thr[:, e:e + 1], in1=gm[:, e, :n], op0=ALU.is_ge, op1=ALU.mult)
            for e in range(2):
                for f2 in range(FF // 256):
                    wgt = P.pick("wgb", wgb)
                    wut = P.pick("wub", wub)
                    P.dma("pool", wgt[:], wg[e * D:(e + 1) * D, f2 * 256:(f2 + 1) * 256].rearrange("(k p) n -> p k n", p=128),
                          reads=[wg], writes=[wgt])
                    P.dma("pool", wut[:], wu[e * D:(e + 1) * D, f2 * 256:(f2 + 1) * 256].rearrange("(k p) n -> p k n", p=128),
                          reads=[wu], writes=[wut])
                    for fs in range(2):
                        pg = P.pick("psg", psg)
                        pu = P.pick("psu", psu)
                        for k in range(32):
                            P.op("pe", "matmul", [wgt, ug], [pg], pg[:, :n], lhsT=wgt[:, k, fs * 128:(fs + 1) * 128],
                                 rhs=ug[:, k, :n], start=(k == 0), stop=(k == 31))
                        for k in range(32):
                            P.op("pe", "matmul", [wut, ug], [pu], pu[:, :n], lhsT=wut[:, k, fs * 128:(fs + 1) * 128],
                                 rhs=ug[:, k, :n], start=(k == 0), stop=(k == 31))
                        sg = P.pick("sg", sgs)
                        P.op("act", "activation", [pg], [sg], out=sg[:, :n], in_=pg[:, :n], func=AF.Silu)
                        P.op("dve", "tensor_tensor", [sg, pu], [sg], out=sg[:, :n], in0=sg[:, :n], in1=pu[:, :n], op=ALU.mult)
                        j = e * 12 + f2 * 2 + fs
                        P.op("pool", "tensor_tensor", [sg, gm], [hdnT], out=hdnT[:, j, :n], in0=sg[:, :n], in1=gm[:, e, :n],
                             op=ALU.mult)
            for dt_ in range(D // 512):
                wdt = P.pick("wdb", wdb)
                for e in range(2):
                    P.dma("pool", wdt[:, e * 12:(e + 1) * 12, :],
                          wd[e * FF:(e + 1) * FF, dt_ * 512:(dt_ + 1) * 512].rearrange("(j p) n -> p j n", p=128),
                          reads=[wd], writes=[wdt])
                for tt in range((n + 127) // 128):
                    nr = min(128, n - tt * 128)
                    pd = P.pick("psd", psd)
                    for j in range(24):
                        P.op("pe", "matmul", [hdnT, wdt], [pd], pd[:nr, :], lhsT=hdnT[:, j, tt * 128:tt * 128 + nr],
                             rhs=wdt[:, j, :], start=(j == 0), stop=(j == 23))
                    ob = P.pick("ob", obufs)
                    copy_op(P, P.pick("oev", ["act", "dve"]), ob[:nr, :], pd[:nr, :], [pd], [ob])
                    P.dma("sp", y[t0 + tt * 128:t0 + tt * 128 + nr, dt_ * 512:(dt_ + 1) * 512], ob[:nr, :], reads=[ob],
                          multi=outs)
        P.wait_multi("sp", outs)
        P.emit()
    return nc


NTT = (L + 127) // 128
FN_SCALE = 1.0 / math.sqrt(L * 256.0)


def build_k9():
    nc = bass.Bass("TRN2", target_bir_lowering=False)
    with ExitStack() as st:
        P = Prog(nc, st)
        ufnT = P.din("ufnT", [1024, L])
        cs256 = P.din("cs256", [256, 512])
        CL = P.din("CL", [L, LT])
        SLn = P.din("SLn", [L, LT])
        yfn = P.dout("yfn", [LT, 1024])
        C = setup_common(P, n_psum=8)
        outs = Multi()
        csb = P.sb("csb", [128, 2, 512], BF16)
        P.dma("pool", csb[:], cs256[:, :].rearrange("(k p) n -> p k n", p=128), reads=[cs256], writes=[csb])
        ug = P.sb("ug", [128, 2, L], BF16)
        vg = P.sb("vg", [128, NTT, 512], BF16)
        cls = [P.sb("cl%d" % i, [128, NTT, 128], BF16) for i in range(2)]
        sls = [P.sb("sl%d" % i, [128, NTT, 128], BF16) for i in range(2)]
        obufs = [P.sb("ob%d" % i, [128, 256], F32) for i in range(2)]
        for g in range(4):
            P.dma("pool", ug[:], ufnT[g * 256:(g + 1) * 256, :].rearrange("(k p) n -> p k n", p=128), reads=[ufnT], writes=[ug])
            for tt in range(NTT):
                nr = min(128, L - tt * 128)
                ps = P.pick("vps", C.ps[0:4])
                for k in range(2):
                    P.op("pe", "matmul", [ug, csb], [ps], ps[:nr, :], lhsT=ug[:, k, tt * 128:tt * 128 + nr], rhs=csb[:, k, :],
                         start=(k == 0), stop=(k == 1))
                copy_op(P, P.pick("vev", ["act", "dve"]), vg[:nr, tt, :], ps[:nr, :], [ps], [vg])
            for i in range(NT):
                r0, nr = trows(i)
                cl = P.pick("cl", cls)
                sl = P.pick("sl", sls)
                for (dst, src) in ((cl, CL), (sl, SLn)):
                    P.dma("pool", dst[:, 0:NTT - 1, :nr], src[0:(NTT - 1) * 128, r0:r0 + nr].rearrange("(t p) n -> p t n", p=128),
                          reads=[src], writes=[dst])
                    P.dma("pool", dst[:16, NTT - 1, :nr], src[(NTT - 1) * 128:L, r0:r0 + nr], reads=[src], writes=[dst])
                ps = P.pick("yps", C.ps[4:8])
                for tt in range(NTT):
                    kr = min(128, L - tt * 128)
                    P.op("pe", "matmul", [cl, vg], [ps], ps[:nr, 0:256], lhsT=cl[:kr, tt, :nr], rhs=vg[:kr, tt, 0:256],
                         start=(tt == 0), stop=False)
                    P.op("pe", "matmul", [sl, vg], [ps], ps[:nr, 0:256], lhsT=sl[:kr, tt, :nr], rhs=vg[:kr, tt, 256:512],
                         start=False, stop=(tt == NTT - 1))
                ob = P.pick("ob", obufs)
                P.op("act", "mul", [ps], [ob], ob[:nr, :], ps[:nr, 0:256], FN_SCALE)
                P.dma("sp", yfn[r0:r0 + nr, g * 256:(g + 1) * 256], ob[:nr, :], reads=[ob], multi=outs)
        P.wait_multi("sp", outs)
        P.emit()
    return nc


def k9_consts(c):
    cc = np.arange(256, dtype=np.int64)
    ang = 2.0 * np.pi * ((cc[:, None] * cc[None, :]) % 256) / 256.0
    cs256 = np.concatenate([np.cos(ang), np.sin(ang)], axis=1).astype(np.float32)
    t = np.arange(L, dtype=np.int64)
    k = np.concatenate([np.arange(16), 16 + c * TPC + np.arange(TPC)]).astype(np.int64)
    angL = 2.0 * np.pi * ((t[:, None] * k[None, :]) % L) / float(L)
    return cs256, np.cos(angL).astype(np.float32), (-np.sin(angL)).astype(np.float32)


def build_k8():
    nc = bass.Bass("TRN2", target_bir_lowering=False)
    with ExitStack() as st:
        P = Prog(nc, st)
        qT = P.din("qT", [256, LP])
        kT = P.din("kT", [256, LP])
        ktok = P.din("ktok", [LP, 256])
        vv = P.din("v", [LP, 512])
        gates = P.din("gates", [128, 4 * NSLOT])
        hp = P.din("hp", [1, 4])
        cst = P.din("cst", [128, 5 * 128 + NSLOT])
        houts = [P.dout("hf", [LP, 512]), P.dout("hb", [LP, 512])]
        C = setup_common(P, n_psum=8)
        psm, pR, pE, pqk, pnum, pint, pcl0, pcl1 = C.ps
        outs = Multi()
        cs = P.sb("cs", [128, 5 * 128 + NSLOT], F32)
        P.dma("sp", cs[:], cst[:, :], reads=[cst], writes=[cs])
        TRIs = [cs[:, 0:128], cs[:, 128:256]]
        LOW = cs[:, 256:384]
        UPP = cs[:, 384:512]
        ONES = cs[:, 512:640]
        valid = cs[:, 640:640 + NSLOT]
        onesb = P.sb("onesb", [128, 2], BF16)
        P.op("dve", "memset", [], [onesb], onesb[:], 1.0)
        gt = P.sb("gt", [128, 4, NSLOT], F32)
        P.dma("sp", gt[:].rearrange("p a s -> p (a s)"), gates[:, :], reads=[gates], writes=[gt])
        hpb = P.sb("hpb", [128, 4], F32)
        load_bcast_row(P, "sp", hpb, hp[0:1, :], hp)
        t1 = P.sb("t1", [128, NSLOT], F32)
        for d in range(2):
            li = gt[:, 2 * d, :]
            lf = gt[:, 2 * d + 1, :]
            P.op("dve", "tensor_scalar", [gt, hpb], [gt], out=li, in0=li, scalar1=hpb[:, 2 * d:2 * d + 1], scalar2=None,
                 op0=ALU.add)
            P.op("dve", "tensor_tensor", [gt, cs], [gt], out=li, in0=li, in1=valid, op=ALU.mult)
            P.op("dve", "tensor_scalar", [gt, hpb], [gt], out=lf, in0=lf, scalar1=hpb[:, 2 * d + 1:2 * d + 2], scalar2=None,
                 op0=ALU.add)
            P.op("dve", "scalar_tensor_tensor", [gt], [t1], out=t1[:], in0=lf, scalar=-1.0, in1=lf, op0=ALU.mult, op1=ALU.max)
            P.op("act", "activation", [t1], [t1], out=t1[:], in_=t1[:], func=AF.Exp, scale=-1.0)
            P.op("act", "activation", [t1], [t1], out=t1[:], in_=t1[:], func=AF.Ln, bias=1.0)
            P.op("dve", "scalar_tensor_tensor", [gt, t1], [gt], out=lf, in0=lf, scalar=0.0, in1=t1[:], op0=ALU.min,
                 op1=ALU.subtract)
            P.op("dve", "tensor_tensor", [gt, cs], [gt], out=lf, in0=lf, in1=valid, op=ALU.mult)
        Cst = P.sb("Cst", [128, 2, 512], F32)
        nst = P.sb("nst", [128, 2], F32)
        mst = P.sb("mst", [128, 1], F32)
        Cb = P.sb("Cb", [128, 2, 512], BF16)
        nb = P.sb("nb", [128, 2], BF16)
        qTs = [P.sb("qTc%d" % i, [128, 2, 128], BF16) for i in range(2)]
        kTs = [P.sb("kTc%d" % i, [128, 2, 128], BF16) for i in range(2)]
        kts = [P.sb("ktc%d" % i, [128, 256], BF16) for i in range(2)]
        vcs = [P.sb("vc%d" % i, [128, 512], BF16) for i in range(2)]
        s8s = [P.sb("s8_%d" % i, [128, 20], F32) for i in range(2)]
        dgm = P.sb("dgm", [128, 128], F32)
        dgm2 = P.sb("dgm2", [128, 128], F32)
        ET = P.sb("ET", [128, 128], F32)
        pTs = [P.sb("pT%d" % i, [128, 128], BF16) for i in range(2)]
        ints = P.sb("ints", [128, 512], F32)
        nums = P.sb("nums", [128, 512], F32)
        hsbs = [P.sb("hsb%d" % i, [128, 512], F32) for i in range(2)]
        kws = [P.sb("kw%d" % i, [128, 256], BF16) for i in range(2)]
        tmpC = P.sb("tmpC", [128, 512], F32)
        nl = P.sb("nl", [128, 2], F32)

        def col(s, j):
            return s[:, j:j + 1]

        for d in range(2):
            TRI = TRIs[d]
            MR1 = LOW if d == 0 else UPP
            ME = UPP if d == 0 else LOW
            P.op("dve", "memset", [], [Cst], Cst[:], 0.0)
            P.op("dve", "memset", [], [nst], nst[:], 0.0)
            P.op("dve", "memset", [], [mst], mst[:], 0.0)
            P.op("pool", "memset", [], [Cb], Cb[:], 0.0)
            P.op("pool", "memset", [], [nb], nb[:], 0.0)
            order = range(NSLOT) if d == 0 else range(NSLOT - 1, -1, -1)
            for ci, c in enumerate(order):
                t0 = c * 128
                qTc = qTs[ci % 2]
                kTc = kTs[ci % 2]
                ktc = kts[ci % 2]
                vc = vcs[ci % 2]
                s = s8s[ci % 2]
                pT = pTs[ci % 2]
                hsb = hsbs[ci % 2]
                kw = kws[ci % 2]
                li = gt[:, 2 * d, c:c + 1]
                lf = gt[:, 2 * d + 1, c:c + 1]
                P.dma("pool", qTc[:], qT[:, t0:t0 + 128].rearrange("(k p) n -> p k n", p=128), reads=[qT], writes=[qTc])
                P.dma("pool", kTc[:], kT[:, t0:t0 + 128].rearrange("(k p) n -> p k n", p=128), reads=[kT], writes=[kTc])
                P.dma("pool", ktc[:], ktok[t0:t0 + 128, :], reads=[ktok], writes=[ktc])
                P.dma("pool", vc[:], vv[t0:t0 + 128, :], reads=[vv], writes=[vc])
                P.op("pe", "matmul", [cs, gt], [psm], psm[:, 0:1], lhsT=TRI, rhs=lf, start=True, stop=True, skip_group_check=True)
                P.op("pe", "matmul", [cs, gt], [psm], psm[:, 1:2], lhsT=ONES, rhs=lf, start=True, stop=True, skip_group_check=True)
                P.op("act", "copy", [psm], [s], s[:, 0:2], psm[:, 0:2])
                P.op("dve", "tensor_tensor", [gt, s], [s], out=col(s, 2), in0=li, in1=col(s, 0), op=ALU.subtract)
                P.op("dve", "tensor_scalar", [C.identf, s], [dgm], out=dgm[:], in0=C.identf[:], scalar1=col(s, 2), scalar2=None,
                     op0=ALU.mult)
                P.op("pe", "matmul", [cs, dgm], [pR], pR[:, 0:128], lhsT=ONES, rhs=dgm[:], start=True, stop=True,
                     skip_group_check=True)
                P.op("pe", "matmul", [cs, dgm], [pR], pR[:, 128:256], lhsT=ONES, rhs=dgm[:], start=True, stop=False,
                     skip_group_check=True)
                P.op("pe", "matmul", [cs, C.identf], [pR], pR[:, 128:256], lhsT=C.identf[:], rhs=MR1, start=False, stop=True,
                     skip_group_check=True)
                P.op("dve", "reduce_max", [pR], [s], out=col(s, 3), in_=pR[:, 0:128], axis=AX.X)
                P.op("dve", "reduce_max", [pR], [s], out=col(s, 4), in_=pR[:, 128:256], axis=AX.X)
                P.op("dve", "tensor_tensor", [s, mst], [s], out=col(s, 5), in0=col(s, 4), in1=mst[:, 0:1], op=ALU.max)
                P.op("dve", "tensor_scalar", [s], [s], out=col(s, 6), in0=col(s, 5), scalar1=-1.0, scalar2=None, op0=ALU.mult)
                P.op("dve", "tensor_tensor", [s], [s], out=col(s, 7), in0=col(s, 0), in1=col(s, 5), op=ALU.add)
                P.op("dve", "tensor_tensor", [s, mst], [s], out=col(s, 14), in0=mst[:, 0:1], in1=col(s, 6), op=ALU.add)
                P.op("act", "activation", [s], [s], out=col(s, 8), in_=col(s, 14), func=AF.Exp)
                P.op("dve", "tensor_scalar", [C.identf, s], [dgm2], out=dgm2[:], in0=C.identf[:], scalar1=col(s, 6), scalar2=None,
                     op0=ALU.mult)
                P.op("pe", "matmul", [cs, dgm2], [pE], pE[:, 0:128], lhsT=ONES, rhs=dgm2[:], start=True, stop=False,
                     skip_group_check=True)
                P.op("pe", "matmul", [cs, C.identf], [pE], pE[:, 0:128], lhsT=C.identf[:], rhs=ME, start=False, stop=True,
                     skip_group_check=True)
                P.op("act", "activation", [pE, s], [ET], out=ET[:], in_=pE[:, 0:128], func=AF.Exp, bias=col(s, 2))
                for kd in range(2):
                    P.op("pe", "matmul", [kTc, qTc], [pqk], pqk[:, 0:128], lhsT=kTc[:, kd, :], rhs=qTc[:, kd, :],
                         start=(kd == 0), stop=(kd == 1))
                P.op("dve", "tensor_tensor", [ET, pqk], [pT], out=pT[:], in0=ET[:], in1=pqk[:, 0:128], op=ALU.mult)
                P.op("pe", "matmul", [pT, vc], [pnum], pnum[:, :], lhsT=pT[:], rhs=vc[:], start=True, stop=True)
                P.op("pe", "matmul", [pT, onesb], [psm], psm[:, 2:3], lhsT=pT[:], rhs=onesb[:, 0:1], start=True, stop=True,
                     skip_group_check=True)
                for kd in range(2):
                    P.op("pe", "matmul", [qTc, Cb], [pint], pint[:, :], lhsT=qTc[:, kd, :], rhs=Cb[:, kd, :],
                         start=(kd == 0), stop=(kd == 1))
                for kd in range(2):
                    P.op("pe", "matmul", [qTc, nb], [psm], psm[:, 3:4], lhsT=qTc[:, kd, :], rhs=nb[:, kd:kd + 1],
                         start=(kd == 0), stop=(kd == 1), skip_group_check=True)
                P.op("act", "copy", [pint], [ints], ints[:], pint[:, :])
                P.op("dve", "scalar_tensor_tensor", [ints, s, pnum], [nums], out=nums[:], in0=ints[:], scalar=col(s, 8),
                     in1=pnum[:, :], op0=ALU.mult, op1=ALU.add)
                P.op("act", "copy", [psm], [s], s[:, 16:18], psm[:, 2:4])
                P.op("dve", "scalar_tensor_tensor", [s], [s], out=col(s, 15), in0=col(s, 17), scalar=col(s, 8), in1=col(s, 16),
                     op0=ALU.mult, op1=ALU.add)
                P.op("dve", "scalar_tensor_tensor", [s], [s], out=col(s, 14), in0=col(s, 15), scalar=-1.0, in1=col(s, 15),
                     op0=ALU.mult, op1=ALU.max)
                P.op("act", "activation", [s], [s], out=col(s, 18), in_=col(s, 7), func=AF.Exp, scale=-1.0)
                P.op("dve", "scalar_tensor_tensor", [s], [s], out=col(s, 14), in0=col(s, 18), scalar=16.0, in1=col(s, 14),
                     op0=ALU.mult, op1=ALU.max)
                P.op("dve", "reciprocal", [s], [s], out=col(s, 19), in_=col(s, 14))
                P.op("act", "mul", [nums, s], [hsb], hsb[:], nums[:], col(s, 19))
                P.dma("sp", houts[d][t0:t0 + 128, :], hsb[:], reads=[hsb], multi=outs)
                P.op("dve", "tensor_tensor", [s], [s], out=col(s, 14), in0=col(s, 2), in1=col(s, 3), op=ALU.subtract)
                P.op("act", "activation", [s], [s], out=col(s, 9), in_=col(s, 14), func=AF.Exp)
                P.op("dve", "tensor_scalar", [ktc, s], [kw], out=kw[:], in0=ktc[:], scalar1=col(s, 9), scalar2=None, op0=ALU.mult)
                P.op("pe", "matmul", [kw, vc], [pcl0], pcl0[:, :], lhsT=kw[:, 0:128], rhs=vc[:], start=True, stop=True)
                P.op("pe", "matmul", [kw, vc], [pcl1], pcl1[:, :], lhsT=kw[:, 128:256], rhs=vc[:], start=True, stop=True)
                for kd in range(2):
                    P.op("pe", "matmul", [kw, onesb], [psm], psm[:, 4 + kd:5 + kd], lhsT=kw[:, kd * 128:(kd + 1) * 128],
                         rhs=onesb[:, 0:1], start=True, stop=True, skip_group_check=True)
                P.op("dve", "tensor_tensor", [s], [s], out=col(s, 10), in0=col(s, 1), in1=col(s, 3), op=ALU.add)
                P.op("dve", "tensor_tensor", [s, mst], [s], out=col(s, 14), in0=col(s, 1), in1=mst[:, 0:1], op=ALU.add)
                P.op("dve", "tensor_tensor", [s], [s], out=col(s, 11), in0=col(s, 14), in1=col(s, 10), op=ALU.max)
                P.op("dve", "tensor_tensor", [s], [s], out=col(s, 14), in0=col(s, 14), in1=col(s, 11), op=ALU.subtract)
                P.op("act", "activation", [s], [s], out=col(s, 12), in_=col(s, 14), func=AF.Exp)
                P.op("dve", "tensor_tensor", [s], [s], out=col(s, 14), in0=col(s, 10), in1=col(s, 11), op=ALU.subtract)
                P.op("act", "activation", [s], [s], out=col(s, 13), in_=col(s, 14), func=AF.Exp)
                for kd, pcl in ((0, pcl0), (1, pcl1)):
                    P.op("act", "mul", [pcl, s], [tmpC], tmpC[:], pcl[:, :], col(s, 13))
                    P.op("dve", "scalar_tensor_tensor", [Cst, s, tmpC], [Cst], out=Cst[:, kd, :], in0=Cst[:, kd, :],
                         scalar=col(s, 12), in1=tmpC[:], op0=ALU.mult, op1=ALU.add)
                P.op("act", "mul", [psm, s], [nl], nl[:], psm[:, 4:6], col(s, 13))
                P.op("dve", "scalar_tensor_tensor", [nst, s, nl], [nst], out=nst[:], in0=nst[:], scalar=col(s, 12), in1=nl[:],
                     op0=ALU.mult, op1=ALU.add)
                P.op("act", "copy", [Cst], [Cb], Cb[:].rearrange("p k v -> p (k v)"), Cst[:].rearrange("p k v -> p (k v)"))
                P.op("dve", "tensor_copy", [nst], [nb], nb[:], nst[:])
                P.op("dve", "tensor_copy", [s], [mst], mst[:, 0:1], col(s, 11))
        P.wait_multi("sp", outs)
        P.emit()
    return nc


def k8_consts():
    k = np.arange(128)[:, None]
    l = np.arange(128)[None, :]
    tri_f = (k <= l).astype(np.float32)
    tri_b = (k >= l).astype(np.float32)
    low = np.where(l <= k, 0.0, NEG).astype(np.float32)
    upp = np.where(l >= k, 0.0, NEG).astype(np.float32)
    valid = np.ones((128, NSLOT), np.float32)
    valid[:112, 0] = 0.0
    return np.concatenate([tri_f, tri_b, low, upp, np.ones((128, 128), np.float32), valid], axis=1)


def k8_host_inputs(proj, i_bias, f_bias, hd, cst):
    def pos(a):
        out = np.zeros((LP,) + a.shape[1:], np.float32)
        out[112:] = a
        return out
    q = pos(proj[:, hd * 256:(hd + 1) * 256])
    k = pos(proj[:, 1536 + hd * 256:1536 + (hd + 1) * 256])
    v = pos(proj[:, 3072 + hd * 512:3072 + (hd + 1) * 512])
    ip = pos(proj[:, 9216:9228].reshape(L, 2, 6)[:, :, hd])
    fp = pos(proj[:, 9228:9240].reshape(L, 2, 6)[:, :, hd])
    def tm(a):
        return a.reshape(NSLOT, 128).T
    gates = np.concatenate([tm(ip[:, 0]), tm(fp[:, 0]), tm(ip[:, 1]), tm(fp[:, 1])], axis=1)
    hp = np.array([[i_bias[0, hd], f_bias[0, hd], i_bias[1, hd], f_bias[1, hd]]], np.float32)
    return {"qT": np.ascontiguousarray(q.T), "kT": np.ascontiguousarray(k.T), "ktok": k, "v": v,
            "gates": np.ascontiguousarray(gates, dtype=np.float32), "hp": hp, "cst": cst}


_PROGS = {}


def _prog(key, fn):
    if key not in _PROGS:
        _PROGS[key] = fn()
    return _PROGS[key]


_T0 = [None]


def _run(nc, in_maps):
    import sys
    import time
    if _T0[0] is None:
        _T0[0] = time.time()
    t = time.time()
    res = run_bass_kernel_spmd(nc, in_maps, core_ids=list(range(NCORE))).results
    sys.stderr.write("[kernel] launch done: %.1fs (t=%.1fs)\n" % (time.time() - t, time.time() - _T0[0]))
    sys.stderr.flush()
    return res


def _loc(a, c):
    return np.ascontiguousarray(np.concatenate([a[:NMETA], a[NMETA + c * TPC:NMETA + (c + 1) * TPC]], axis=0))


def _glob(parts):
    return np.concatenate([parts[0][:NMETA]] + [p[NMETA:] for p in parts], axis=0)


def _f32(a):
    return np.ascontiguousarray(a, dtype=np.float32)


def _moe(u2T_parts, aff_parts, wg, wu, wd):
    u2T = np.concatenate([u2T_parts[0][:, :NMETA]] + [p[:, NMETA:] for p in u2T_parts], axis=1)
    u2 = np.ascontiguousarray(u2T.T)
    aff = _glob(aff_parts)
    nc = _prog("k7s", build_k7s)
    cst = k7s_consts()
    in_maps = []
    for c in range(NCORE):
        es = [2 * c, 2 * c + 1]
        in_maps.append({"u2": u2, "affT": np.ascontiguousarray(aff[:, es].T), "afftm": k7s_afftm(aff[:, es]), "cst": cst,
                        "wg": np.ascontiguousarray(wg[es]).reshape(2 * D, FF),
                        "wu": np.ascontiguousarray(wu[es]).reshape(2 * D, FF),
                        "wd": np.ascontiguousarray(wd[es]).reshape(2 * FF, D)})
    res = _run(nc, in_maps)
    return [r["y"] for r in res]


def kernel(x, meta_tokens, norm_mix, ab_w_in, ab_conv_w, ab_conv_b, ab_a_log, ab_dt_bias, ab_d_skip,
           ab_ssd_norm, ab_sink, ab_w_out, cd_w_in, cd_i_bias, cd_f_bias, cd_head_norm, cd_w_out,
           norm_ffn, moe_router, moe_w_gate, moe_w_up, moe_w_down, final_norm):
    x = _f32(x)[0]
    meta = _f32(meta_tokens)
    hfull = np.concatenate([meta, x], axis=0)
    nc = _prog("k1_l0", lambda: build_k1(1, 8256))
    res = _run(nc, [{"part0": _loc(hfull, c), "g": _f32(norm_mix[0:1]), "w": _f32(ab_w_in[0])} for c in range(NCORE)])
    proj = _glob([r["out"] for r in res])
    nc = _prog("k2", build_k2)
    res = _run(nc, [k2_host_inputs(proj, _f32(ab_conv_w[0]), _f32(ab_conv_b[0]), _f32(ab_a_log[0]), _f32(ab_dt_bias[0]), c)
                    for c in range(NCORE)])
    yf = np.concatenate([k2_unslot(res[2 * g]["y"], 0) for g in range(4)], axis=1)
    yb = np.concatenate([k2_unslot(res[2 * g + 1]["y"], 1) for g in range(4)], axis=1)
    xs = np.concatenate([k2_unslot(res[2 * g]["xs"], 0) for g in range(4)], axis=1)
    nc = _prog("k3", build_k3)
    cst3 = k3_consts()
    res = _run(nc, [k3_host_inputs(proj, _f32(ab_sink[0]), c, cst3) for c in range(NCORE)])
    yatt = _glob([r["yatt"] for r in res])
    vec = np.concatenate([np.repeat(_f32(ab_d_skip[0]), 64), _f32(ab_ssd_norm[0])])[None, :]
    nc = _prog("k4_0", lambda: build_k4(0))
    res = _run(nc, [{"yf": _loc(yf, c), "yb": _loc(yb, c), "xs": _loc(xs, c), "z": _loc(proj[:, :2048], c),
                     "yatt": _loc(yatt, c), "vec": _f32(vec), "h": _loc(hfull, c), "g2": _f32(norm_ffn[0:1]),
                     "wout": _f32(ab_w_out[0]), "wr": _f32(moe_router[0])} for c in range(NCORE)])
    h1 = [r["h1"] for r in res]
    ymoe = _moe([r["u2T"] for r in res], [r["aff"] for r in res], moe_w_gate[0], moe_w_up[0], moe_w_down[0])
    del proj, yf, yb, xs, yatt
    nc = _prog("k1_l1", lambda: build_k1(9, 10264))
    in_maps = []
    for c in range(NCORE):
        m = {"part0": h1[c], "g": _f32(norm_mix[1:2]), "w": _f32(cd_w_in[0])}
        for e in range(NCORE):
            m["part%d" % (e + 1)] = _loc(ymoe[e], c)
        in_maps.append(m)
    res = _run(nc, in_maps)
    del ymoe, in_maps
    proj = _glob([r["out"] for r in res])
    h2 = [r["hout"] for r in res]
    nc = _prog("k8", build_k8)
    cst8 = k8_consts()
    res = _run(nc, [k8_host_inputs(proj, _f32(cd_i_bias[0]), _f32(cd_f_bias[0]), c % 6, cst8) for c in range(NCORE)])
    hf = np.concatenate([res[hd]["hf"][112:] for hd in range(6)], axis=1)
    hb = np.concatenate([res[hd]["hb"][112:] for hd in range(6)], axis=1)
    nc = _prog("k9", build_k9)
    ufnT = np.ascontiguousarray(proj[:, 9240:10264].T)
    in_maps = []
    for c in range(NCORE):
        cs, cl, sl = k9_consts(c)
        in_maps.append({"ufnT": ufnT, "cs256": cs, "CL": cl, "SLn": sl})
    res = _run(nc, in_maps)
    yfn = [r["yfn"] for r in res]
    nc = _prog("k4_1", lambda: build_k4(1))
    res = _run(nc, [{"hf": _loc(hf, c), "hb": _loc(hb, c), "opre": _loc(proj[:, 6144:9216], c), "yfn": yfn[c],
                     "vec": _f32(cd_head_norm[0:1]), "h": h2[c], "g2": _f32(norm_ffn[1:2]),
                     "wout": _f32(cd_w_out[0]), "wr": _f32(moe_router[1])} for c in range(NCORE)])
    h1 = [r["h1"] for r in res]
    ymoe = _moe([r["u2T"] for r in res], [r["aff"] for r in res], moe_w_gate[1], moe_w_up[1], moe_w_down[1])
    del proj, hf, hb
    nc = _prog("k1_fin", lambda: build_k1(9, 0, matmul=False, final=True))
    in_maps = []
    for c in range(NCORE):
        m = {"part0": h1[c], "g": _f32(final_norm[None, :])}
        for e in range(NCORE):
            m["part%d" % (e + 1)] = _loc(ymoe[e], c)
        in_maps.append(m)
    res = _run(nc, in_maps)
    out = np.concatenate([r["fout"][NMETA:] for r in res], axis=0)
    return np.ascontiguousarray(out[None], dtype=np.float32)


I32 = mybir.dt.int32
NJ = (L + 127) // 128
BIGPOS = float(CAP)


def build_k7s():
    nc = bass.Bass("TRN2", target_bir_lowering=False)
    with ExitStack() as st:
        P = Prog(nc, st)
        u2 = P.din("u2", [L, D], BF16)
        affT = P.din("affT", [2, L])
        afftm = P.din("afftm", [128, 2 * NJ])
        cst = P.din("cst", [128, 128 + NJ])
        wg = P.din("wg", [2 * D, FF])
        wu = P.din("wu", [2 * D, FF])
        wd = P.din("wd", [2 * FF, D])
        y = P.dout("y", [L, D])
        xs_d = [P.dscr("xs_d%d" % e, [CAP + 1, D], BF16) for e in range(2)]
        oe_d = [P.dscr("oe_d%d" % e, [CAP + 1, D], F32) for e in range(2)]
        C = setup_common(P, n_psum=7)
        pbf = P.ps("pbf", [128, 1024], BF16)
        outs = Multi()
        thr = P.sb("thr", [128, 2], F32)
        cs = P.sb("cs", [128, 128 + NJ], F32)
        P.dma("sp", cs[:], cst[:, :], reads=[cst], writes=[cs])
        atm = P.sb("atm", [128, 2, NJ], F32)
        P.dma("sp", atm[:].rearrange("p e j -> p (e j)"), afftm[:, :], reads=[afftm], writes=[atm])
        gate = P.sb("gate", [128, 2, NJ], F32)
        posi = [P.sb("posi%d" % e, [128, NJ], I32) for e in range(2)]
        ones1 = P.sb("ones1", [128, 128], F32)
        P.op("dve", "memset", [], [ones1], ones1[:], 1.0)
        with P.scope():
            arow = P.sb("arow", [128, L], F32)
            junk = P.sb("junk", [128, L], BF16)
            b = P.sb("bis", [128, 8], F32)
            for e in range(2):
                P.dma("sp", arow[:], affT[e:e + 1, :].partition_broadcast(128), reads=[affT], writes=[arow])
                P.op("dve", "memset", [], [b], b[:, 0:1], 0.0)
                P.op("dve", "memset", [], [b], b[:, 1:2], 1.0)
                for it in range(32):
                    P.op("dve", "tensor_tensor", [b], [b], out=b[:, 2:3], in0=b[:, 0:1], in1=b[:, 1:2], op=ALU.add)
                    P.op("dve", "tensor_scalar", [b], [b], out=b[:, 2:3], in0=b[:, 2:3], scalar1=0.5, scalar2=None,
                         op0=ALU.mult)
                    P.op("dve", "memset", [], [b], b[:, 3:4], 0.0)
                    P.op("dve", "tensor_scalar", [arow, b], [junk, b], out=junk[:], in0=arow[:], scalar1=b[:, 2:3],
                         scalar2=0.0, op0=ALU.is_ge, op1=ALU.add, accum_out=b[:, 3:4])
                    P.op("dve", "tensor_scalar", [b], [b], out=b[:, 4:5], in0=b[:, 3:4], scalar1=float(CAP) - 0.5,
                         scalar2=None, op0=ALU.is_ge)
                    P.op("dve", "tensor_scalar", [b], [b], out=b[:, 6:7], in0=b[:, 4:5], scalar1=-1.0, scalar2=1.0,
                         op0=ALU.mult, op1=ALU.add)
                    P.op("dve", "tensor_tensor", [b], [b], out=b[:, 5:6], in0=b[:, 2:3], in1=b[:, 0:1], op=ALU.subtract)
                    P.op("dve", "scalar_tensor_tensor", [b], [b], out=b[:, 0:1], in0=b[:, 5:6], scalar=b[:, 4:5],
                         in1=b[:, 0:1], op0=ALU.mult, op1=ALU.add)
                    P.op("dve", "tensor_tensor", [b], [b], out=b[:, 5:6], in0=b[:, 2:3], in1=b[:, 1:2], op=ALU.subtract)
                    P.op("dve", "scalar_tensor_tensor", [b], [b], out=b[:, 1:2], in0=b[:, 5:6], scalar=b[:, 6:7],
                         in1=b[:, 1:2], op0=ALU.mult, op1=ALU.add)
                P.op("dve", "tensor_copy", [b], [thr], thr[:, e:e + 1], b[:, 0:1])
        mask = P.sb("mask", [128, 2, NJ], F32)
        tcol = P.sb("tcol", [128, 2], F32)
        tb = P.sb("tb", [128, 128], F32)
        posf = P.sb("posf", [128, 2, NJ], F32)
        pw, po_, pt_ = C.ps[0], C.ps[1], C.ps[2]
        for e in range(2):
            P.op("dve", "tensor_scalar", [atm, thr], [mask], out=mask[:, e, :], in0=atm[:, e, :], scalar1=thr[:, e:e + 1],
                 scalar2=None, op0=ALU.is_ge)
            P.op("dve", "tensor_tensor", [atm, mask], [gate], out=gate[:, e, :], in0=atm[:, e, :], in1=mask[:, e, :], op=ALU.mult)
            P.op("pe", "matmul", [cs, mask], [pw], pw[:, 0:NJ], lhsT=cs[:, 0:128], rhs=mask[:, e, :], start=True, stop=True)
            P.op("pe", "matmul", [mask, ones1], [pt_], pt_[:NJ, 0:1], lhsT=mask[:, e, :], rhs=ones1[:, 0:1], start=True, stop=True)
            P.op("act", "copy", [pt_], [tcol], tcol[:NJ, e:e + 1], pt_[:NJ, 0:1])
            P.op("dve", "tensor_scalar", [ones1, tcol], [tb], out=tb[:NJ, :], in0=ones1[:NJ, :], scalar1=tcol[:NJ, e:e + 1],
                 scalar2=None, op0=ALU.mult)
            P.op("pe", "matmul", [tb, cs], [po_], po_[:, 0:NJ], lhsT=tb[:NJ, :], rhs=cs[:NJ, 128:128 + NJ], start=True, stop=True)
            P.op("act", "copy", [pw], [posf], posf[:, e, :], pw[:, 0:NJ])
            P.op("dve", "tensor_tensor", [posf, po_], [posf], out=posf[:, e, :], in0=posf[:, e, :], in1=po_[:, 0:NJ], op=ALU.add)
            P.op("dve", "tensor_scalar", [posf], [posf], out=posf[:, e, :], in0=posf[:, e, :], scalar1=-1.0 - BIGPOS, scalar2=None,
                 op0=ALU.add)
            P.op("dve", "tensor_tensor", [posf, mask], [posf], out=posf[:, e, :], in0=posf[:, e, :], in1=mask[:, e, :], op=ALU.mult)
            P.op("dve", "tensor_scalar", [posf], [posf], out=posf[:, e, :], in0=posf[:, e, :], scalar1=BIGPOS, scalar2=None,
                 op0=ALU.add)
            P.op("dve", "tensor_copy", [posf], [posi[e]], posi[e][:, :], posf[:, e, :])
        scat = [Multi(), Multi()]
        with P.scope():
            ubufs = [P.sb("ub%d" % i, [128, D], BF16) for i in range(3)]
            for j in range(NJ):
                nr = min(128, L - j * 128)
                ub = P.pick("ub", ubufs)
                P.dma("sp", ub[:nr, :], u2[j * 128:j * 128 + nr, :], reads=[u2], writes=[ub])
                for e in range(2):
                    P.idma("pool", out=xs_d[e][:, :], out_offset=bass.IndirectOffsetOnAxis(ap=posi[e][:, j:j + 1], axis=0),
                           in_=ub[:, :], in_offset=None, reads=[ub, posi[e]], multi=scat[e])
        oew = [Multi(), Multi()]
        with P.scope():
            xT = P.sb("xT", [128, 32, TG], BF16)
            hdnT = P.sb("hdnT", [128, 12, TG], BF16)
            xrow = [P.sb("xrow%d" % i, [128, D], BF16) for i in range(2)]
            wgb = [P.sb("wgb%d" % i, [128, 32, 256], BF16) for i in range(2)]
            wub = [P.sb("wub%d" % i, [128, 32, 256], BF16) for i in range(2)]
            wdb = [P.sb("wdb%d" % i, [128, 12, 512], BF16) for i in range(2)]
            sgs = [P.sb("sg%d" % i, [128, TG], F32) for i in range(2)]
            obufs = [P.sb("ob%d" % i, [128, 512], F32) for i in range(4)]
            psg = C.ps[0:2]
            psu = C.ps[2:4]
            psd = C.ps[4:7]
            for e in range(2):
                for g0 in range(0, CAP, TG):
                    n = min(TG, CAP - g0)
                    for tt in range((n + 127) // 128):
                        nr = min(128, n - tt * 128)
                        xr = P.pick("xrow", xrow)
                        P.dma("sp", xr[:nr, :], xs_d[e][g0 + tt * 128:g0 + tt * 128 + nr, :], reads=[xs_d[e]], writes=[xr],
                              after=scat[e])
                        for k8 in range(0, 32, 8):
                            for k in range(8):
                                P.op("pe", "transpose", [xr, C.identb], [pbf], pbf[:, k * 128:k * 128 + nr],
                                     xr[:nr, (k8 + k) * 128:(k8 + k + 1) * 128], C.identb[:nr, :nr])
                            copy_op(P, P.pick("tev", ["act", "dve"]), xT[:, k8:k8 + 8, tt * 128:tt * 128 + nr],
                                    pbf[:, :].rearrange("p (k n) -> p k n", k=8)[:, :, :nr], [pbf], [xT])
                    for f2 in range(FF // 256):
                        wgt = P.pick("wgb", wgb)
                        wut = P.pick("wub", wub)
                        P.dma("pool", wgt[:], wg[e * D:(e + 1) * D, f2 * 256:(f2 + 1) * 256].rearrange("(k p) n -> p k n", p=128),
                              reads=[wg], writes=[wgt])
                        P.dma("pool", wut[:], wu[e * D:(e + 1) * D, f2 * 256:(f2 + 1) * 256].rearrange("(k p) n -> p k n", p=128),
                              reads=[wu], writes=[wut])
                        for fs in range(2):
                            pg = P.pick("psg", psg)
                            pu = P.pick("psu", psu)
                            for k in range(32):
                                P.op("pe", "matmul", [wgt, xT], [pg], pg[:, :n], lhsT=wgt[:, k, fs * 128:(fs + 1) * 128],
                                     rhs=xT[:, k, :n], start=(k == 0), stop=(k == 31))
                            for k in range(32):
                                P.op("pe", "matmul", [wut, xT], [pu], pu[:, :n], lhsT=wut[:, k, fs * 128:(fs + 1) * 128],
                                     rhs=xT[:, k, :n], start=(k == 0), stop=(k == 31))
                            sg = P.pick("sg", sgs)
                            P.op("act", "activation", [pg], [sg], out=sg[:, :n], in_=pg[:, :n], func=AF.Silu)
                            P.op("dve", "tensor_tensor", [sg, pu], [hdnT], out=hdnT[:, f2 * 2 + fs, :n], in0=sg[:, :n],
                                 in1=pu[:, :n], op=ALU.mult)
                    for dt_ in range(D // 512):
                        wdt = P.pick("wdb", wdb)
                        P.dma("pool", wdt[:], wd[e * FF:(e + 1) * FF, dt_ * 512:(dt_ + 1) * 512].rearrange("(j p) n -> p j n", p=128),
                              reads=[wd], writes=[wdt])
                        for tt in range((n + 127) // 128):
                            nr = min(128, n - tt * 128)
                            pd = P.pick("psd", psd)
                            for j in range(12):
                                P.op("pe", "matmul", [hdnT, wdt], [pd], pd[:nr, :], lhsT=hdnT[:, j, tt * 128:tt * 128 + nr],
                                     rhs=wdt[:, j, :], start=(j == 0), stop=(j == 11))
                            ob = P.pick("ob", obufs)
                            copy_op(P, P.pick("oev", ["act", "dve"]), ob[:nr, :], pd[:nr, :], [pd], [ob])
                            P.dma("sp", oe_d[e][g0 + tt * 128:g0 + tt * 128 + nr, dt_ * 512:(dt_ + 1) * 512], ob[:nr, :],
                                  reads=[ob], multi=oew[e])
        zrow = P.sb("zrow", [1, D], F32)
        P.op("dve", "memset", [], [zrow], zrow[:], 0.0)
        for e in range(2):
            P.dma("sp", oe_d[e][CAP:CAP + 1, :], zrow[:], reads=[zrow], multi=oew[e])
        gts = [P.sb("gt%d" % i, [128, D], F32) for i in range(4)]
        yts = [P.sb("yt%d" % i, [128, D], F32) for i in range(2)]
        for j in range(NJ):
            nr = min(128, L - j * 128)
            yt = yts[j % 2]
            for e in range(2):
                gt_ = P.pick("gts", gts)
                P.idma("pool", out=gt_[:, :], out_offset=None, in_=oe_d[e][:, :],
                       in_offset=bass.IndirectOffsetOnAxis(ap=posi[e][:, j:j + 1], axis=0),
                       reads=[posi[e], oe_d[e]], writes=[gt_], after=oew[e])
                if e == 0:
                    P.op("dve", "tensor_scalar", [gt_, gate], [yt], out=yt[:nr, :], in0=gt_[:nr, :], scalar1=gate[:nr, 0, j:j + 1],
                         scalar2=None, op0=ALU.mult)
                else:
                    P.op("dve", "scalar_tensor_tensor", [gt_, gate, yt], [yt], out=yt[:nr, :], in0=gt_[:nr, :],
                         scalar=gate[:nr, 1, j:j + 1], in1=yt[:nr, :], op0=ALU.mult, op1=ALU.add)
            P.dma("sp", y[j * 128:j * 128 + nr, :], yt[:nr, :], reads=[yt], multi=outs)
        P.wait_multi("sp", outs)
        P.emit()
    return nc


def k7s_consts():
    k = np.arange(128)[:, None]
    p = np.arange(128)[None, :]
    tri = (k <= p).astype(np.float32)
    strict = np.zeros((128, NJ), np.float32)
    jj = np.arange(NJ)
    strict[:NJ, :] = (jj[:, None] < jj[None, :]).astype(np.float32)
    return np.concatenate([tri, strict], axis=1)


def k7s_afftm(aff2):
    pad = np.zeros((NJ * 128, 2), np.float32)
    pad[:L] = aff2
    return np.ascontiguousarray(pad.reshape(NJ, 128, 2).transpose(1, 2, 0).reshape(128, 2 * NJ))
```
